# Optimizing a Trainium2 kernel written in Bass

```python
import jax, jax.numpy as jnp
from jax import lax
import numpy as np

D_MODEL = 1024
BATCH = 2
SEQ = 8192
DEPTH = 2

GRID_W = 64
CTX_LEN = 256
NA_HEADS = 8
NA_HEAD_DIM = 64
NA_ROWS = 8
NA_COLS = 16
NA_QCB = 32
NA_KCB = NA_QCB + NA_COLS
HG_HEADS = 4
HG_DK = 128
HG_DV = 128
HG_CHUNK = 64
LOG_FLOOR = 1e-30
WA_HEADS = 8
WA_KV_HEADS = 2
WA_HEAD_DIM = 64
WA_WINDOW = 128
WA_BLOCK = 128
ROPE_THETA = 10000.0
N_EXPERTS = 16
N_GROUPS = 4
EXPERTS_PER_GROUP = N_EXPERTS // N_GROUPS
TOP_K = 2
EXPERT_FF = 512
MOE_BLOCK = 128
N_BRANCHES = 3
NA_W = NA_HEADS * NA_HEAD_DIM
HG_KW = HG_HEADS * HG_DK
HG_VW = HG_HEADS * HG_DV
WA_QW = WA_HEADS * WA_HEAD_DIM
WA_KVW = WA_KV_HEADS * WA_HEAD_DIM
IN_SPLITS = (NA_W, NA_W, NA_W, HG_KW, HG_KW, HG_KW, HG_VW, HG_VW, WA_QW, WA_KVW, WA_KVW, N_BRANCHES * D_MODEL)
IN_COLS = sum(IN_SPLITS)
NEG_INF = -1e30
RMS_EPS = 1e-6

kernel_name = 'hybrid_natten_hgrn2_swa_moe_dit'


def rms_norm(x, w):
    xf = x.astype(jnp.float32)
    y = xf * lax.rsqrt(jnp.mean(xf * xf, axis=-1, keepdims=True) + RMS_EPS)
    return (y * w.astype(jnp.float32)).astype(x.dtype)


def modulation(cond, w, b):
    return jnp.split(jax.nn.silu(cond) @ w + b, 6, axis=-1)


def modulate(h, shift, scale):
    return h * (1 + scale) + shift


def rope_2d(x, row_pos, col_pos):
    half = x.shape[-1] // 2
    quarter = half // 2
    inv_freq = ROPE_THETA ** (-jnp.arange(quarter, dtype=jnp.float32) / quarter)

    def rotate(xp, pos):
        ang = pos[:, None] * inv_freq[None, :]
        cos = jnp.cos(ang)[None, :, None, :]
        sin = jnp.sin(ang)[None, :, None, :]
        x1 = xp[..., :quarter].astype(jnp.float32)
        x2 = xp[..., quarter:].astype(jnp.float32)
        return jnp.concatenate([x1 * cos - x2 * sin, x2 * cos + x1 * sin], axis=-1)

    out = jnp.concatenate([rotate(x[..., :half], row_pos), rotate(x[..., half:], col_pos)], axis=-1)
    return out.astype(x.dtype)


def context_attention(q, k, v, sink):
    B, L, H, Dh = q.shape
    Hkv = k.shape[2]
    G = H // Hkv
    qg = q.reshape(B, L, Hkv, G, Dh)
    s = jnp.einsum('blkgd,bmkd->bkglm', qg, k).astype(jnp.float32) * (Dh ** -0.5)
    if sink is not None:
        s_sink = jnp.broadcast_to(sink.astype(jnp.float32).reshape(1, Hkv, G, 1, 1), s.shape[:-1] + (1,))
        s = jnp.concatenate([s, s_sink], axis=-1)
    p = jax.nn.softmax(s, axis=-1)[..., :L].astype(v.dtype)
    o = jnp.einsum('bkglm,bmkd->blkgd', p, v)
    return o.reshape(B, L, H * Dh)


def neighbourhood_attention(q, k, v, kc, vc, rpb):
    B, T, H, Dh = q.shape
    rows = T // GRID_W
    kr = min(NA_ROWS, rows)
    ncb = GRID_W // NA_QCB
    r = np.arange(rows)
    row_idx = np.clip(r - kr // 2, 0, rows - kr)[:, None] + np.arange(kr)[None, :]
    qcol = np.arange(GRID_W).reshape(ncb, NA_QCB)
    col_lo = np.clip(qcol - NA_COLS // 2, 0, GRID_W - NA_COLS)
    blk_lo = np.minimum(col_lo[:, 0], GRID_W - NA_KCB)
    col_idx = blk_lo[:, None] + np.arange(NA_KCB)[None, :]
    key_col = col_idx[:, None, :]
    col_ok = (key_col >= col_lo[..., None]) & (key_col < col_lo[..., None] + NA_COLS)
    d_row = row_idx - r[:, None] + NA_ROWS - 1
    d_col = np.clip(key_col - qcol[..., None] + NA_COLS - 1, 0, 2 * NA_COLS - 2)
    bias = rpb[:, d_row[:, None, None, :, None], d_col[None, :, :, None, :]]

    kg = k.reshape(B, rows, GRID_W, H, Dh)
    vg = v.reshape(B, rows, GRID_W, H, Dh)
    gi_r = row_idx[:, :, None, None]
    gi_c = col_idx[None, None, :, :]
    k_nb = kg[:, gi_r, gi_c]
    v_nb = vg[:, gi_r, gi_c]
    qb = q.reshape(B, rows, ncb, NA_QCB, H, Dh)
    scale = Dh ** -0.5
    s_nb = jnp.einsum('brjqhd,brajmhd->bhrjqam', qb, k_nb).astype(jnp.float32) * scale + bias[None].astype(jnp.float32)
    s_nb = jnp.where(col_ok[:, :, None, :], s_nb, NEG_INF)
    n_nb = kr * NA_KCB
    s_nb = s_nb.reshape(B, H, rows, ncb, NA_QCB, n_nb)
    s_ctx = jnp.einsum('brjqhd,blhd->bhrjql', qb, kc).astype(jnp.float32) * scale
    p = jax.nn.softmax(jnp.concatenate([s_nb, s_ctx], axis=-1), axis=-1).astype(v.dtype)
    p_nb = p[..., :n_nb].reshape(B, H, rows, ncb, NA_QCB, kr, NA_KCB)
    o = (jnp.einsum('bhrjqam,brajmhd->brjqhd', p_nb, v_nb)
         + jnp.einsum('bhrjql,blhd->brjqhd', p[..., n_nb:], vc))
    return o.reshape(B, T, H * Dh)


def window_attention(q, k, v, kc, vc, sink):
    B, T, H, Dh = q.shape
    Hkv = k.shape[2]
    G = H // Hkv
    L = kc.shape[1]
    nb = T // WA_BLOCK
    qb = q.reshape(B, nb, WA_BLOCK, Hkv, G, Dh)

    def band(a):
        ap = jnp.pad(a, ((0, 0), (WA_BLOCK, WA_BLOCK), (0, 0), (0, 0))).reshape(B, nb + 2, WA_BLOCK, Hkv, Dh)
        return jnp.concatenate([ap[:, :-2], ap[:, 1:-1], ap[:, 2:]], axis=2)

    kb = band(k)
    vb = band(v)
    blk = np.arange(nb)[:, None, None] * WA_BLOCK
    qpos = blk + np.arange(WA_BLOCK)[None, :, None]
    kpos = blk - WA_BLOCK + np.arange(3 * WA_BLOCK)[None, None, :]
    ok = (np.abs(kpos - qpos) <= WA_WINDOW) & (kpos >= 0) & (kpos < T)
    scale = Dh ** -0.5
    s_loc = jnp.einsum('bnqkgd,bnmkd->bkgnqm', qb, kb).astype(jnp.float32) * scale
    s_loc = jnp.where(ok, s_loc, NEG_INF)
    s_ctx = jnp.einsum('bnqkgd,blkd->bkgnql', qb, kc).astype(jnp.float32) * scale
    s_sink = jnp.broadcast_to(sink.astype(jnp.float32).reshape(1, Hkv, G, 1, 1, 1), s_ctx.shape[:-1] + (1,))
    p = jax.nn.softmax(jnp.concatenate([s_loc, s_ctx, s_sink], axis=-1), axis=-1).astype(v.dtype)
    nloc = 3 * WA_BLOCK
    o = (jnp.einsum('bkgnqm,bnmkd->bnqkgd', p[..., :nloc], vb)
         + jnp.einsum('bkgnql,blkd->bnqkgd', p[..., nloc:nloc + L], vc))
    return o.reshape(B, T, H * Dh)


def gla_chunk_scan(q, k, v, log_f, s0):
    B, T, H, dk = q.shape
    dv = v.shape[-1]
    C = HG_CHUNK
    n = T // C

    def chunks(a):
        return a.reshape(B, n, C, H, a.shape[-1]).transpose(1, 0, 3, 2, 4)

    lower_tri = np.tril(np.ones((C, C), dtype=bool))[:, :, None]

    def step(S, inp):
        qc, kc, vc, gc = inp
        b = jnp.cumsum(gc, axis=2)
        diff = b[:, :, :, None, :] - b[:, :, None, :, :]
        decay = jnp.exp(jnp.where(lower_tri, diff, NEG_INF))
        att = jnp.einsum('bhik,bhijk,bhjk->bhij', qc, decay, kc)
        o = jnp.einsum('bhij,bhjv->bhiv', att, vc) + jnp.einsum('bhik,bhkv->bhiv', qc * jnp.exp(b), S)
        b_last = b[:, :, -1:, :]
        S_new = (jnp.exp(b_last[:, :, 0, :])[..., None] * S
                 + jnp.einsum('bhjk,bhjv->bhkv', kc * jnp.exp(b_last - b), vc))
        return S_new, o

    s_final, o = lax.scan(step, s0, (chunks(q), chunks(k), chunks(v), chunks(log_f)))
    return o.transpose(1, 0, 3, 2, 4).reshape(B, T, H, dv), s_final


def hgrn_lower_bounds(hg_lower):
    p = jax.nn.softmax(hg_lower.astype(jnp.float32), axis=1)
    return jnp.cumsum(p, axis=1) - p[:, :1]


def hgrn2_mixer(q, f_fwd, f_bwd, i, g, lb_fwd, lb_bwd, norm_w, n_ctx):
    B, N = q.shape[:2]
    qf = jax.nn.silu(q.astype(jnp.float32))
    vf = i.astype(jnp.float32)
    s0 = jnp.zeros((B, HG_HEADS, HG_DK, HG_DV), jnp.float32)
    outs = []
    for f_pre, lb, reverse in ((f_fwd, lb_fwd, False), (f_bwd, lb_bwd, True)):
        lbh = lb.reshape(HG_HEADS, HG_DK)
        f = lbh + (1.0 - lbh) * jax.nn.sigmoid(f_pre.astype(jnp.float32))
        k = 1.0 - f
        log_f = jnp.log(jnp.maximum(f, LOG_FLOOR))
        orient = (lambda a: jnp.flip(a, axis=1)) if reverse else (lambda a: a)
        seqs = (qf, k, vf, log_f)
        o_c, s_c = gla_chunk_scan(*[orient(a[:, :n_ctx]) for a in seqs], s0)
        o_l, _ = gla_chunk_scan(*[orient(a[:, n_ctx:]) for a in seqs], s_c)
        outs.append(jnp.concatenate([orient(o_c), orient(o_l)], axis=1))
    o = rms_norm(outs[0] + outs[1], norm_w) * jax.nn.silu(g.astype(jnp.float32))
    return o.reshape(B, N, HG_VW).astype(q.dtype)


def token_mixers(h, n_ctx, need_ctx, w_in, na_qn, na_kn, na_rpb, lb_fwd, lb_bwd, hg_norm,
                 wa_qn, wa_kn, wa_sink, w_pa, w_pb, w_pc, w_out, row_pos, col_pos):
    B, N, D = h.shape
    L = n_ctx
    cuts = np.cumsum(IN_SPLITS)[:-1].tolist()
    qa, ka, va, qh, fh_f, fh_b, ih, gh, qw, kw, vw, gates = jnp.split(h @ w_in, cuts, axis=-1)

    def heads(a, n):
        return a.reshape(B, N, n, -1)

    qa = rms_norm(heads(qa, NA_HEADS), na_qn)
    ka = rms_norm(heads(ka, NA_HEADS), na_kn)
    va = heads(va, NA_HEADS)
    y_a = neighbourhood_attention(qa[:, L:], ka[:, L:], va[:, L:], ka[:, :L], va[:, :L], na_rpb)
    y_b = hgrn2_mixer(heads(qh, HG_HEADS), heads(fh_f, HG_HEADS), heads(fh_b, HG_HEADS),
                      heads(ih, HG_HEADS), heads(gh, HG_HEADS), lb_fwd, lb_bwd, hg_norm, L)
    qw = rms_norm(heads(qw, WA_HEADS), wa_qn)
    kw = rms_norm(heads(kw, WA_KV_HEADS), wa_kn)
    vw = heads(vw, WA_KV_HEADS)
    y_c = window_attention(rope_2d(qw[:, L:], row_pos, col_pos), rope_2d(kw[:, L:], row_pos, col_pos),
                           vw[:, L:], kw[:, :L], vw[:, :L], wa_sink)
    if need_ctx:
        y_a = jnp.concatenate([context_attention(qa[:, :L], ka[:, :L], va[:, :L], None), y_a], axis=1)
        y_c = jnp.concatenate([context_attention(qw[:, :L], kw[:, :L], vw[:, :L], wa_sink), y_c], axis=1)
    else:
        y_b = y_b[:, L:]
        gates = gates[:, L:]
    g_a, g_b, g_c = jnp.split(jax.nn.sigmoid(gates), N_BRANCHES, axis=-1)
    merged = g_a * (y_a @ w_pa) + g_b * (y_b @ w_pb) + g_c * (y_c @ w_pc)
    return merged @ w_out


def moe_ffn(h, w_router, b_router, w_gate, w_up, w_down):
    N, D = h.shape
    probs = jax.nn.softmax((h @ w_router).astype(jnp.float32), axis=-1)
    sel = (probs + b_router.astype(jnp.float32)).reshape(N, N_GROUPS, EXPERTS_PER_GROUP)
    grp_score = lax.top_k(sel, TOP_K)[0].sum(-1)
    g_idx = jnp.argmax(grp_score, axis=-1)
    in_grp = jnp.take_along_axis(sel, g_idx[:, None, None], axis=1)[:, 0]
    _, loc = lax.top_k(in_grp, TOP_K)
    e_idx = g_idx[:, None] * EXPERTS_PER_GROUP + loc
    wts = jnp.take_along_axis(probs, e_idx, axis=1)
    wts = wts / jnp.sum(wts, axis=-1, keepdims=True)
    A = N * TOP_K
    flat_e = e_idx.reshape(A)
    flat_tok = jnp.repeat(jnp.arange(N), TOP_K)
    flat_w = wts.reshape(A)
    order = jnp.argsort(flat_e)
    se, st, sw = flat_e[order], flat_tok[order], flat_w[order]
    counts = jnp.bincount(flat_e, length=N_EXPERTS)
    padded = ((counts + MOE_BLOCK - 1) // MOE_BLOCK) * MOE_BLOCK
    pend = jnp.cumsum(padded)
    pstart = pend - padded
    start = jnp.cumsum(counts) - counts
    dest = pstart[se] + jnp.arange(A) - start[se]
    nblk = -(-A // MOE_BLOCK) + N_EXPERTS
    buf_tok = jnp.zeros((nblk * MOE_BLOCK,), jnp.int32).at[dest].set(st)
    blk_e = jnp.minimum(jnp.searchsorted(pend, jnp.arange(nblk) * MOE_BLOCK, side='right'), N_EXPERTS - 1)
    xb = h[buf_tok].reshape(nblk, MOE_BLOCK, D)

    def expert_block(args):
        xblk, e = args
        return (jax.nn.silu(xblk @ w_gate[e]) * (xblk @ w_up[e])) @ w_down[e]

    yb = lax.map(expert_block, (xb, blk_e)).reshape(nblk * MOE_BLOCK, D)
    y = yb[dest] * sw[:, None].astype(h.dtype)
    return jax.ops.segment_sum(y, st, num_segments=N)


def hybrid_layer(x, ctx, c, c_ctx, need_ctx, w_ada, b_ada, norm1, norm2, w_in, na_qn, na_kn, na_rpb,
                 lb_fwd, lb_bwd, hg_norm, wa_qn, wa_kn, wa_sink, w_pa, w_pb, w_pc, w_out,
                 w_router, b_router, w_gate, w_up, w_down, row_pos, col_pos):
    B, T, D = x.shape
    L = ctx.shape[1]
    m = modulation(c, w_ada, b_ada)
    mc = modulation(c_ctx, w_ada, b_ada)
    h = jnp.concatenate([modulate(rms_norm(ctx, norm1), mc[0], mc[1]),
                         modulate(rms_norm(x, norm1), m[0][:, None], m[1][:, None])], axis=1)
    mix = token_mixers(h, L, need_ctx, w_in, na_qn, na_kn, na_rpb, lb_fwd, lb_bwd, hg_norm,
                       wa_qn, wa_kn, wa_sink, w_pa, w_pb, w_pc, w_out, row_pos, col_pos)
    if need_ctx:
        ctx = ctx + mc[2] * mix[:, :L]
        x = x + m[2][:, None] * mix[:, L:]
        hc = modulate(rms_norm(ctx, norm2), mc[3], mc[4])
        hx = modulate(rms_norm(x, norm2), m[3][:, None], m[4][:, None])
        y = moe_ffn(jnp.concatenate([hc, hx], axis=1).reshape(-1, D), w_router, b_router,
                    w_gate, w_up, w_down).reshape(B, L + T, D)
        ctx = ctx + mc[5] * y[:, :L]
        x = x + m[5][:, None] * y[:, L:]
    else:
        x = x + m[2][:, None] * mix
        hx = modulate(rms_norm(x, norm2), m[3][:, None], m[4][:, None])
        y = moe_ffn(hx.reshape(-1, D), w_router, b_router, w_gate, w_up, w_down).reshape(B, T, D)
        x = x + m[5][:, None] * y
    return x, ctx


def setup_inputs(seed: int = 0) -> dict:
    key = jax.random.key(seed)
    ks = jax.random.split(key, 26)
    f32 = jnp.float32
    D = D_MODEL

    def nrm(k, shape, scale):
        return jax.random.normal(k, shape, f32) * scale

    return {
        'x': nrm(ks[0], (BATCH, SEQ, D), 1.0),
        'c': nrm(ks[1], (BATCH, D), 1.0),
        'ctx': nrm(ks[2], (BATCH, CTX_LEN, D), 1.0),
        'c_ctx': nrm(ks[3], (D,), 1.0),
        'w_ada': nrm(ks[4], (DEPTH, D, 6 * D), 0.5 * D ** -0.5),
        'b_ada': nrm(ks[5], (DEPTH, 6 * D), 0.01),
        'norm1': 1.0 + nrm(ks[6], (DEPTH, D), 0.01),
        'norm2': 1.0 + nrm(ks[7], (DEPTH, D), 0.01),
        'w_in': nrm(ks[8], (DEPTH, D, IN_COLS), D ** -0.5),
        'na_q_norm': 1.0 + nrm(ks[9], (DEPTH, NA_HEAD_DIM), 0.01),
        'na_k_norm': 1.0 + nrm(ks[10], (DEPTH, NA_HEAD_DIM), 0.01),
        'na_rpb': nrm(ks[11], (DEPTH, NA_HEADS, 2 * NA_ROWS - 1, 2 * NA_COLS - 1), 0.2),
        'hg_lower': nrm(ks[12], (2, DEPTH, HG_KW), 1.0),
        'hg_norm': 1.0 + nrm(ks[13], (DEPTH, HG_DV), 0.01),
        'wa_q_norm': 1.0 + nrm(ks[14], (DEPTH, WA_HEAD_DIM), 0.01),
        'wa_k_norm': 1.0 + nrm(ks[15], (DEPTH, WA_HEAD_DIM), 0.01),
        'wa_sink': nrm(ks[16], (DEPTH, WA_HEADS), 1.0),
        'w_pa': nrm(ks[17], (DEPTH, NA_W, D), NA_W ** -0.5),
        'w_pb': nrm(ks[18], (DEPTH, HG_VW, D), HG_VW ** -0.5),
        'w_pc': nrm(ks[19], (DEPTH, WA_QW, D), WA_QW ** -0.5),
        'w_out': nrm(ks[20], (DEPTH, D, D), D ** -0.5),
        'w_router': nrm(ks[21], (D, N_EXPERTS), D ** -0.5),
        'b_router': nrm(ks[22], (N_EXPERTS,), 0.01),
        'w_gate': nrm(ks[23], (DEPTH, N_EXPERTS, D, EXPERT_FF), D ** -0.5),
        'w_up': nrm(ks[24], (DEPTH, N_EXPERTS, D, EXPERT_FF), D ** -0.5),
        'w_down': nrm(ks[25], (DEPTH, N_EXPERTS, EXPERT_FF, D), EXPERT_FF ** -0.5),
    }


def reference(x, c, ctx, c_ctx, w_ada, b_ada, norm1, norm2, w_in, na_q_norm, na_k_norm, na_rpb, hg_lower,
              hg_norm, wa_q_norm, wa_k_norm, wa_sink, w_pa, w_pb, w_pc, w_out, w_router, b_router,
              w_gate, w_up, w_down):
    T = x.shape[1]
    pos = jnp.arange(T)
    row_pos = (pos // GRID_W).astype(jnp.float32)
    col_pos = (pos % GRID_W).astype(jnp.float32)
    lb = hgrn_lower_bounds(hg_lower)
    for l in range(DEPTH):
        x, ctx = hybrid_layer(x, ctx, c, c_ctx, l < DEPTH - 1, w_ada[l], b_ada[l], norm1[l], norm2[l], w_in[l],
                              na_q_norm[l], na_k_norm[l], na_rpb[l], lb[0, l], lb[1, l], hg_norm[l],
                              wa_q_norm[l], wa_k_norm[l], wa_sink[l], w_pa[l], w_pb[l], w_pc[l], w_out[l],
                              w_router, b_router, w_gate[l], w_up[l], w_down[l], row_pos, col_pos)
    return x
```

```python
import contextlib
import numpy as np
import concourse.bass as bass
import concourse.mybir as mybir
from concourse.bass_utils import run_bass_kernel_spmd

F32 = mybir.dt.float32
BF16 = mybir.dt.bfloat16
AF = mybir.ActivationFunctionType
ALU = mybir.AluOpType
AX = mybir.AxisListType
ENGS = ("pe", "act", "dve", "pool", "sp")
RMS_EPS = 1e-6
NCORES = 8


class Res:
    __slots__ = ("w", "r")

    def __init__(self):
        self.w = None
        self.r = []


class Op:
    __slots__ = ("eng", "fn", "deps", "is_dma", "needed", "sem", "val", "waits", "wkey", "is_bar", "ep_switch")

    def __init__(self, eng, fn, is_dma):
        self.eng, self.fn, self.is_dma = eng, fn, is_dma
        self.deps = set()
        self.needed = False
        self.sem = None
        self.val = None
        self.waits = []
        self.wkey = None
        self.is_bar = False
        self.ep_switch = False


class Prog:
    N_DMA_SEMS = 15
    N_EPOCHS = 3

    def __init__(self, nc):
        self.nc = nc
        self.ops = {e: [] for e in ENGS}
        self.all_ops = []

    def op(self, eng, fn, reads=(), writes=(), dma=False):
        o = Op(eng, fn, dma)
        for r in reads:
            if r.w is not None:
                o.deps.add(r.w)
        for w in writes:
            if w.w is not None:
                o.deps.add(w.w)
            o.deps.update(w.r)
        for r in reads:
            r.r.append(o)
        for w in writes:
            w.w = o
            w.r = []
        o.deps.discard(o)
        o.wkey = id(writes[0]) if len(writes) else None
        self.ops[eng].append(o)
        self.all_ops.append(o)
        return o

    def dma(self, eng, out, in_, reads=(), writes=(), **kw):
        return self.op(eng, lambda e: e.dma_start(out=out, in_=in_, **kw), reads, writes, dma=True)

    def mm(self, out, lhsT, rhs, start, stop, reads=(), writes=(), **kw):
        return self.op("pe", lambda e: e.matmul(out, lhsT, rhs, start=start, stop=stop, **kw), reads, writes)

    def allgather(self, out, in_, reads=(), writes=()):
        return self.op("pool", lambda e: e.collective_compute("AllGather", ALU.bypass, replica_groups=[list(range(NCORES))],
                                                             ins=[in_], outs=[out]), reads, writes, dma="cc")

    def fence(self, eng, reads):
        return self.op(eng, None, reads, ())

    def emit(self):
        nc = self.nc
        for o in self.all_ops:
            for d in o.deps:
                if d.is_dma or not (d.eng == o.eng == "pe" and not o.is_dma):
                    d.needed = True
        with contextlib.ExitStack() as stack:
            NE = self.N_EPOCHS
            esem = {(e, ep): stack.enter_context(nc.semaphore("s_%s%d" % (e, ep))) for e in ENGS for ep in range(NE)}
            dsem = [[stack.enter_context(nc.semaphore("d_%d_%d" % (ep, i))) for i in range(self.N_DMA_SEMS)] for ep in range(NE)]
            ccsem = stack.enter_context(nc.semaphore("s_cc"))
            cccnt = [0]
            res2sem = {}
            free = list(range(self.N_DMA_SEMS))
            semcnt = [[0] * self.N_DMA_SEMS for _ in range(NE)]
            ecnt = {(e, ep): 0 for e in ENGS for ep in range(NE)}
            rr = 0
            ep = 0
            for o in self.all_ops:
                if o.is_bar:
                    res2sem = {}
                    free = list(range(self.N_DMA_SEMS))
                    if o.ep_switch:
                        ep = min(ep + 1, NE - 1)
                    continue
                if o.is_dma == "cc":
                    cccnt[0] += 1
                    o.sem, o.val = ("cc", "x", 0), cccnt[0]
                elif o.is_dma:
                    k = res2sem.get(o.wkey)
                    if k is None:
                        if free:
                            k = free.pop(0)
                        else:
                            k = rr % self.N_DMA_SEMS
                            rr += 1
                        res2sem[o.wkey] = k
                    semcnt[ep][k] += 16
                    o.sem, o.val = ("d", ep, k), semcnt[ep][k]
                elif o.needed and o.fn is not None:
                    ecnt[(o.eng, ep)] += 1
                    o.sem, o.val = ("e", o.eng, ep), ecnt[(o.eng, ep)]
            self.maxvals = (max(ecnt.values()), max(max(r_) for r_ in semcnt))
            for e in ENGS:
                known = {}
                for o in self.ops[e]:
                    need = {}
                    for d in o.deps:
                        if d.fn is None:
                            continue
                        if (not d.is_dma) and d.eng == e == "pe" and not o.is_dma:
                            continue
                        if known.get(d.sem, 0) >= d.val:
                            continue
                        need[d.sem] = max(need.get(d.sem, 0), d.val)
                    known.update(need)
                    o.waits = sorted(need.items(), key=lambda t: str(t[0]))

            def handle(s):
                if s[0] == "cc":
                    return ccsem
                return esem[(s[1], s[2])] if s[0] == "e" else dsem[s[1]][s[2]]

            def replay(ename):
                def run(eng):
                    for o in self.ops[ename]:
                        for s, v in o.waits:
                            eng.wait_ge(handle(s), v)
                        if o.fn is None:
                            continue
                        ins = o.fn(eng)
                        if o.sem is not None:
                            if o.is_dma == "cc":
                                ins.then_inc(handle(o.sem))
                            else:
                                ins.then_inc(handle(o.sem), 16 if o.is_dma else 1)
                return run

            with nc.Block() as block:
                block.tensor(replay("pe"))
                block.scalar(replay("act"))
                block.vector(replay("dve"))
                block.gpsimd(replay("pool"))
                block.sync(replay("sp"))


class Ctx:
    def __init__(self):
        self.nc = bass.Bass("TRN2", target_bir_lowering=False)
        self.P = Prog(self.nc)
        self.st = contextlib.ExitStack()
        self.outs = []
        self.dq = 0
        self.nps = 0

    def sb(self, name, shape, dt=F32):
        return self.st.enter_context(self.nc.sbuf_tensor(name, shape, dt))

    def ps(self, name=None):
        self.nps += 1
        return self.st.enter_context(self.nc.psum_tensor(name or ("ps%d" % self.nps), [128, 512], F32))

    def din(self, name, shape, dt=F32):
        return self.nc.dram_tensor(name, list(shape), dt, kind="ExternalInput").ap()

    def dout(self, name, shape, dt=F32):
        return self.nc.dram_tensor(name, list(shape), dt, kind="ExternalOutput").ap()

    def ld(self, out, in_, res, eng=None):
        if eng is None:
            eng = ("sp", "pool")[self.dq % 2]
            self.dq += 1
        return self.P.dma(eng, out, in_, writes=[res])

    def store(self, out, in_, res, eng=None):
        if eng is None:
            eng = ("sp", "pool")[self.dq % 2]
            self.dq += 1
        r = Res()
        o = self.P.dma(eng, out, in_, reads=[res], writes=[r])
        self.outs.append(r)
        return o

    def finish(self):
        self.P.fence("sp", self.outs)
        self.P.emit()
        self.st.close()
        return self.nc


NT = 2112
NCTX = 64
N = 8448
L = 256
FM_BLOCKS = [("naq", 0), ("nak", 512), ("hq", 1536), ("ff", 2048), ("fb", 2560), ("hg", 3584), ("waq", 4096)]
FM_ROWS = 7 * 512 + 128
TM_BLOCKS = [("nav", 1024), ("hi", 3072), ("ff", 2048), ("fb", 2560)]
TM_COLS = 4 * 512 + 128
FM_IDX = np.concatenate([np.arange(c, c + 512) for _, c in FM_BLOCKS] + [np.arange(4608, 4736)])
TM_IDX = np.concatenate([np.arange(c, c + 512) for _, c in TM_BLOCKS] + [np.arange(4736, 4864)])
GRP = [[0, 1, 2, 3], [4, 5, 6, 7]]
ALLR = [list(range(8))]
USE_DMA_CAST = True


def barrier(P, ep_switch=False):
    deps = set(o for o in P.all_ops[getattr(P, "bar_idx", 0):] if o.fn is not None)
    for e in ENGS:
        o = Op(e, None, False)
        o.is_bar = True
        o.ep_switch = ep_switch and e == ENGS[0]
        o.deps = set(deps)
        P.ops[e].append(o)
        P.all_ops.append(o)
    P.bar_idx = len(P.all_ops)


class Arena:
    def __init__(self, C, nfloats):
        self.t = C.sb("arena", [128, nfloats], F32)
        self.n = nfloats
        self.off = 0
        self.base = 0

    def _take(self, nf):
        assert self.off + nf <= self.n, ("arena overflow", self.off, nf, self.n)
        v = self.t[:, self.off:self.off + nf]
        self.off += nf
        return v

    def f32(self, shape):
        n = int(np.prod(shape[1:]))
        v = self._take(n)[0:shape[0]]
        if len(shape) == 3:
            v = v.rearrange("p (a b) -> p a b", a=shape[1])
        elif len(shape) == 4:
            v = v.rearrange("p (a b c) -> p a b c", a=shape[1], b=shape[2])
        return v

    def bf16(self, shape):
        n = int(np.prod(shape[1:]))
        nf = (n + 1) // 2
        v = self._take(nf).bitcast(BF16)[0:shape[0], 0:n]
        if len(shape) == 3:
            v = v.rearrange("p (a b) -> p a b", a=shape[1])
        elif len(shape) == 4:
            v = v.rearrange("p (a b c) -> p a b c", a=shape[1], b=shape[2])
        return v

    def reset(self):
        self.off = self.base


class G:
    pass


def ld_w_bf16(g, dst_bf, src_f32, res, stage=None, rstage=None, cast_eng="pool"):
    C, P = g.C, g.P
    if USE_DMA_CAST:
        return P.dma("pool", dst_bf, src_f32, writes=[res])
    C.ld(stage, src_f32, rstage)
    if cast_eng == "act":
        return P.op("act", lambda e: e.copy(dst_bf, stage), [rstage], [res])
    return P.op(cast_eng, lambda e: e.tensor_copy(dst_bf, stage), [rstage], [res])


def rsqrt_ps(g, out, ps, scale, reads, writes_ps, wout):
    P = g.P
    P.op("act", lambda e: e.activation(out, ps, AF.Sqrt, bias=g.eps[0:out.shape[0], :], scale=scale), list(reads) + [g.rconst], list(writes_ps) + [wout])
    P.op("dve", lambda e: e.reciprocal(out, out), [], [wout])


def adaln_phase(g, l):
    C, P, A = g.C, g.P, g.A
    A.reset()
    sc = A.f32([128, 8, 2]); ba = A.f32([128, 48])
    wbuf = [A.f32([128, 8, 512]) for _ in range(2)]; rw = [Res(), Res()]
    rsc, rba = Res(), Res()
    C.ld(ba, g.b_ada[l], rba)
    P.op("act", lambda e: e.activation(sc, g.cv, AF.Silu), [g.rcv], [rsc])
    w_v = g.Wada[l].rearrange("(k p) n -> p k n", p=128)
    ps = g.ps[0]; rps = g.rps[0]
    first = True
    for gi in range(12):
        b = gi % 2
        C.ld(wbuf[b], w_v[:, :, gi * 512:(gi + 1) * 512], rw[b])
        for jj in range(4):
            j = 4 * gi + jj
            for k in range(8):
                P.mm(ps[:, 2 * j:2 * j + 2], wbuf[b][:, k, jj * 128:(jj + 1) * 128], sc[:, k, :],
                     first, k == 7, [rw[b], rsc], [rps], skip_group_check=True)
                first = False
    psv = ps[:, 0:96].rearrange("p (j c) -> p j c", c=2)
    for c in range(2):
        P.op("dve", lambda e, c=c: e.tensor_tensor(g.mod[:, :, c], psv[:, :, c], ba, ALU.add), [rba], [rps, g.rmod])
    for (Aab, Bab, nw, sh0, sc0) in ((g.A1, g.B1, g.n1w[l], 0, 8), (g.A2, g.B2, g.n2w[l], 24, 32)):
        nwt = A.f32([128, 8]); rnw = Res()
        C.ld(nwt, nw, rnw)
        for c in range(2):
            P.op("dve", lambda e, c=c, Aab=Aab, sc0=sc0: e.tensor_scalar(Aab[:, :, c], g.mod[:, sc0:sc0 + 8, c], 1.0, None, ALU.add), [g.rmod], [g.rAB])
            P.op("dve", lambda e, c=c, Aab=Aab, nwt=nwt: e.tensor_tensor(Aab[:, :, c], Aab[:, :, c], nwt, ALU.mult), [rnw], [g.rAB])
            P.op("dve", lambda e, c=c, Bab=Bab, sh0=sh0: e.tensor_copy(Bab[:, :, c], g.mod[:, sh0:sh0 + 8, c]), [g.rmod], [g.rAB])
    barrier(P)


TILES = [(0, 64, 0)] + [(64 + 512 * i, 512, 1) for i in range(4)]


def norm_mod_tile(g, t0, tn, Aab, Bab, c, h_out, rh, scr, hf_out=None):
    P = g.P
    sq, rsq, rstd, rrstd, tmp, rtmp = scr
    ps_n, rpsn = g.ps[1], g.rps[1]
    x_sb, rx = g.x_sb, g.rx
    P.op("act", lambda e: e.activation(sq[:, :, :tn], x_sb[:, :, t0:t0 + tn], AF.Square), [rx], [rsq])
    for k in range(8):
        P.mm(ps_n[:, :tn], g.ones, sq[:, k, :tn], k == 0, k == 7, [rsq, g.rconst], [rpsn])
    rsqrt_ps(g, rstd[:, :tn], ps_n[:, :tn], 1.0 / 1024, [], [rpsn], rrstd)
    for k in range(8):
        P.op("dve", lambda e, k=k: e.tensor_tensor(tmp[k % 2][:, :tn], x_sb[:, k, t0:t0 + tn], rstd[:, :tn], ALU.mult),
             [rx, rrstd], [rtmp[k % 2]])
        P.op("act", lambda e, k=k: e.activation(h_out[:, k, :tn], tmp[k % 2][:, :tn], AF.Identity,
                                                bias=Bab[:, k, c:c + 1], scale=Aab[:, k, c:c + 1]),
             [rtmp[k % 2], g.rAB], [rh])
        if hf_out is not None:
            P.op("pool", lambda e, k=k: e.tensor_scalar(hf_out[:, k, :tn], tmp[k % 2][:, :tn], Aab[:, k, c:c + 1], Bab[:, k, c:c + 1], ALU.mult, ALU.add),
                 [rtmp[k % 2], g.rAB], [rh])


def norm_scratch(g):
    A = g.A
    return (A.f32([128, 8, 512]), Res(), A.f32([128, 512]), Res(), [A.f32([128, 512]) for _ in range(2)], [Res(), Res()])


def phase_s1(g, l):
    C, P, A = g.C, g.P, g.A
    A.reset()
    scr = norm_scratch(g)
    hT = A.bf16([128, 8, NT]); rh = [Res() for _ in TILES]
    for ti, (t0, tn, c) in enumerate(TILES):
        norm_mod_tile(g, t0, tn, g.A1, g.B1, c, hT[:, :, t0:t0 + tn], rh[ti], scr)
    wb = [A.bf16([128, 8, 512]) for _ in range(2)]; rwb = [Res(), Res()]
    if not USE_DMA_CAST:
        wf = [A.f32([128, 8, 512]) for _ in range(2)]; rwf = [Res(), Res()]
    else:
        wf = [None, None]; rwf = [None, None]
    ost = [A.f32([128, NT]) for _ in range(2)]; rost = [Res(), Res()]
    ott = [A.f32([128, 512]) for _ in range(2)]; rott = [Res(), Res()]
    pp = g.ps[2:6]; rpp = g.rps[2:6]
    wv = g.Wfm[l].rearrange("(k p) n -> p k n", p=128)
    gi = 0
    pi = 0
    mi = 0
    rPfm, rPtm = Res(), Res()
    for g0 in range(0, FM_ROWS, 512):
        b = gi % 2
        gi += 1
        ncol = min(512, FM_ROWS - g0)
        ld_w_bf16(g, wb[b][:, :, :ncol], wv[:, :, g0:g0 + ncol], rwb[b], wf[b], rwf[b])
        for m in range(ncol // 128):
            ob = mi % 2
            mi += 1
            for ti, (t0, tn, c) in enumerate(TILES):
                pb = pi % 4
                pi += 1
                for k in range(8):
                    P.mm(pp[pb][:, :tn], wb[b][:, k, m * 128:(m + 1) * 128], hT[:, k, t0:t0 + tn], k == 0, k == 7,
                         [rwb[b], rh[ti]], [rpp[pb]])
                if pi % 2:
                    P.op("act", lambda e, pb=pb, ob=ob, t0=t0, tn=tn: e.copy(ost[ob][:, t0:t0 + tn], pp[pb][:, :tn]), [], [rpp[pb], rost[ob]])
                else:
                    P.op("dve", lambda e, pb=pb, ob=ob, t0=t0, tn=tn: e.tensor_copy(ost[ob][:, t0:t0 + tn], pp[pb][:, :tn]), [], [rpp[pb], rost[ob]])
            r0 = g0 + m * 128
            P.dma("sp", g.Pfm_mine[r0:r0 + 128, :], ost[ob], reads=[rost[ob]], writes=[rPfm])
    wv = g.Wtm[l].rearrange("(k p) n -> p k n", p=128)
    ttiles = [(0, 64)] + [(64 + 128 * i, 128) for i in range(16)]
    for g0 in range(0, TM_COLS, 512):
        b = gi % 2
        gi += 1
        ncol = min(512, TM_COLS - g0)
        ld_w_bf16(g, wb[b][:, :, :ncol], wv[:, :, g0:g0 + ncol], rwb[b], wf[b], rwf[b])
        for (t0, tn) in ttiles:
            pb = pi % 4
            pi += 1
            ob = mi % 2
            mi += 1
            ti = 0 if t0 < 64 else 1 + (t0 - 64) // 512
            for k in range(8):
                P.mm(pp[pb][:tn, :ncol], hT[:, k, t0:t0 + tn], wb[b][:, k, :ncol], k == 0, k == 7, [rwb[b], rh[ti]], [rpp[pb]])
            if pi % 2:
                P.op("act", lambda e, pb=pb, ob=ob, tn=tn, ncol=ncol: e.copy(ott[ob][:tn, :ncol], pp[pb][:tn, :ncol]), [], [rpp[pb], rott[ob]])
            else:
                P.op("dve", lambda e, pb=pb, ob=ob, tn=tn, ncol=ncol: e.tensor_copy(ott[ob][:tn, :ncol], pp[pb][:tn, :ncol]), [], [rpp[pb], rott[ob]])
            P.dma("sp", g.Ptm_mine[t0:t0 + tn, g0:g0 + ncol], ott[ob][:tn, :ncol], reads=[rott[ob]], writes=[rPtm])
    if g.upto == "s1":
        barrier(P)
        return
    rg1, rg2 = Res(), Res()
    P.op("pool", lambda e: e.collective_compute("AllGather", ALU.bypass, replica_groups=ALLR, ins=[g.Pfm_mine], outs=[g.Pfm_g]), [rPfm], [rg1], dma="cc")
    P.op("pool", lambda e: e.collective_compute("AllGather", ALU.bypass, replica_groups=ALLR, ins=[g.Ptm_mine], outs=[g.Ptm_g]), [rPtm], [rg2], dma="cc")
    barrier(P)
    g.rPg = (rg1, rg2)


def select4(g, items):
    C, P, A = g.C, g.P, g.A
    A.reset()
    maxn = max(int(np.prod(sh[1:])) for _, _, sh in items)
    NS = len(items[0][0])
    cand = [[A.f32([128, maxn]) for _ in range(NS)]] * 2
    rc = [[Res() for _ in range(NS)]] * 2
    acc = [A.f32([128, maxn]) for _ in range(2)]; racc = [Res(), Res()]
    engs = ("dve", "pool")
    for it, (srcs, dst, sh) in enumerate(items):
        b = it % 2
        p = sh[0]
        n = int(np.prod(sh[1:]))

        def view(t):
            v = t[0:p, 0:n]
            if len(sh) == 3:
                v = v.rearrange("p (a b) -> p a b", a=sh[1])
            return v
        for s in range(NS):
            C.ld(view(cand[b][s]), srcs[s], rc[b][s], ("sp", "pool")[s % 2])
        en = "dve"
        P.op(en, lambda e, b=b, p=p, n=n: e.tensor_scalar(acc[b][0:p, 0:n], cand[b][0][0:p, 0:n], g.hmask[0:p, 0:1], None, ALU.mult),
             [rc[b][0], g.rconst], [racc[b]])
        for s in range(1, NS):
            P.op(en, lambda e, b=b, p=p, n=n, s=s: e.scalar_tensor_tensor(acc[b][0:p, 0:n], cand[b][s][0:p, 0:n], g.hmask[0:p, s:s + 1], acc[b][0:p, 0:n], ALU.mult, ALU.add),
                 [rc[b][s], g.rconst], [racc[b]])
        P.dma("sp", dst, view(acc[b]), reads=[racc[b]], writes=[g.rsel])
    barrier(P)


def phase_x1(g):
    items = []
    for bi in range(8):
        nr = 128 if bi < 7 else 64
        for r in range(4):
            srcs = []
            for s8 in range(8):
                s = s8 % 4
                r0 = (4 * (s8 // 4) + r) * FM_ROWS + (bi * 512 + 128 * s if bi < 7 else 3584 + 64 * (s // 2))
                srcs.append(g.Pfm_g[r0:r0 + nr, :])
            d0 = bi * 128
            items.append(([sv[:, 0:64] for sv in srcs], g.S2in_fm[d0:d0 + nr, 64 * r:64 * r + 64], [nr, 64]))
            items.append(([sv[:, 64:NT] for sv in srcs], g.S2in_fm[d0:d0 + nr, L + 2048 * r:L + 2048 * r + 2048], [nr, 2048]))
    for bi in range(5):
        nc_ = 128 if bi < 4 else 64
        for r in range(4):
            srcs_c, srcs_l = [], []
            for s8 in range(8):
                s = s8 % 4
                rr = 4 * (s8 // 4) + r
                c0 = bi * 512 + 128 * s if bi < 4 else 2048 + 64 * (s // 2)
                rows = g.Ptm_g[rr * NT:(rr + 1) * NT, c0:c0 + nc_]
                srcs_c.append(rows[0:64, :])
                srcs_l.append(rows[64:NT, :].rearrange("(n p) c -> p n c", p=128))
            d0 = bi * 128
            items.append((srcs_c, g.S2in_tm[64 * r:64 * r + 64, d0:d0 + nc_], [64, nc_]))
            dl = g.S2in_tm[L + 2048 * r:L + 2048 * r + 2048, d0:d0 + nc_].rearrange("(n p) c -> p n c", p=128)
            for q4 in range(4):
                items.append(([sv[:, 4 * q4:4 * q4 + 4, :] for sv in srcs_l], dl[:, 4 * q4:4 * q4 + 4, :], [128, 4, nc_]))
    select4(g, items)


QT = [(0, 256)] + [(256 + 512 * i, 512) for i in range(16)]


def prep_qk(g, row0, nw_col, dst, rope, scr):
    C, P = g.C, g.P
    raw, rraw, sq, rsq, rstd, rrstd, qn, rqn, t1, rt1, cs, rcs = scr
    ps_a, rpa = g.ps[0], g.rps[0]
    ps_b, rpb = g.ps[1], g.rps[1]
    for ti, (t0, tn) in enumerate(QT):
        b = ti % 2
        C.ld(raw[b][:, :tn], g.S2in_fm[row0:row0 + 64, t0:t0 + tn], rraw[b], "sp")
        P.op("act", lambda e, b=b, tn=tn: e.activation(sq[:, :tn], raw[b][:, :tn], AF.Square), [rraw[b]], [rsq])
        P.mm(ps_a[0:64, :tn], g.ones[0:64, 0:64], sq[:, :tn], True, True, [rsq, g.rconst], [rpa])
        rsqrt_ps(g, rstd[:, :tn], ps_a[0:64, :tn], 1.0 / 64, [], [rpa], rrstd)
        if not rope:
            P.op("dve", lambda e, b=b, tn=tn: e.scalar_tensor_tensor(qn[:, :tn], raw[b][:, :tn], g.pvec[0:64, nw_col:nw_col + 1], rstd[:, :tn], ALU.mult, ALU.mult),
                 [rraw[b], rrstd, g.rpv], [rqn])
            P.op("dve", lambda e, tn=tn, t0=t0: e.tensor_copy(dst[:, t0:t0 + tn], qn[:, :tn]), [rqn], [g.rqk])
        else:
            P.op("dve", lambda e, b=b, tn=tn: e.scalar_tensor_tensor(qn[:, :tn], raw[b][:, :tn], g.pvec[0:64, nw_col:nw_col + 1], rstd[:, :tn], ALU.mult, ALU.mult),
                 [rraw[b], rrstd, g.rpv], [rqn])
            C.ld(cs[b][:, 0, :tn], g.ropeC[:, t0:t0 + tn], rcs[b], "pool")
            C.ld(cs[b][:, 1, :tn], g.ropeS[:, t0:t0 + tn], rcs[b], "pool")
            P.mm(ps_b[0:64, :tn], g.ropeR, qn[:, :tn], True, True, [rqn, g.rpv], [rpb])
            P.op("dve", lambda e, b=b, tn=tn: e.tensor_tensor(t1[:, :tn], qn[:, :tn], cs[b][:, 0, :tn], ALU.mult), [rqn, rcs[b]], [rt1])
            P.op("dve", lambda e, b=b, tn=tn: e.tensor_tensor(qn[:, :tn], ps_b[0:64, :tn], cs[b][:, 1, :tn], ALU.mult), [rcs[b]], [rpb, rqn])
            P.op("dve", lambda e, tn=tn, t0=t0: e.tensor_tensor(dst[:, t0:t0 + tn], t1[:, :tn], qn[:, :tn], ALU.add), [rt1, rqn], [g.rqk])


def prep_v(g, col0, vaug, rv):
    P = g.P
    P.op("pool", lambda e: e.memset(vaug[:, :, 64:128], 1.0), [], [rv])
    src = g.S2in_tm[:, col0:col0 + 64].rearrange("(n p) c -> p n c", p=128)
    for n0 in range(0, 66, 6):
        P.dma("pool", vaug[:, n0:n0 + 6, 0:64], src[:, n0:n0 + 6, :], writes=[rv])


def attn_scratch(g, rope=True):
    A = g.A
    raw = [A.f32([64, 512]) for _ in range(2)]
    cs = [A.f32([64, 2, 512]) for _ in range(2)] if rope else None
    return (raw, [Res(), Res()], A.f32([64, 512]), Res(), A.f32([64, 512]), Res(), A.f32([64, 512]), Res(),
            A.f32([64, 512]), Res(), cs, [Res(), Res()])


def finalize_attn(g, o_ps, rops, tn, yrow0, t0, sink_col, fin):
    P = g.P
    rec, rrec, yo, ryo, cnt = fin
    b = cnt[0] % 2
    cnt[0] += 1
    if sink_col is None:
        P.op("dve", lambda e: e.tensor_scalar(rec[64:128, :tn], o_ps[64:128, :tn], g.zcol[64:128, 0:1], None, ALU.add), [g.rpv], [rops, rrec])
        P.op("dve", lambda e: e.reciprocal(rec[64:128, :tn], rec[64:128, :tn]), [], [rrec])
    else:
        P.op("dve", lambda e: e.tensor_scalar(rec[64:128, :tn], o_ps[64:128, :tn], g.esink[64:128, sink_col:sink_col + 1], None, ALU.add), [g.rpv], [rops, rrec])
        P.op("dve", lambda e: e.reciprocal(rec[64:128, :tn], rec[64:128, :tn]), [], [rrec])
    P.op("dve", lambda e, b=b: e.tensor_tensor(yo[b][:, :tn], o_ps[0:64, :tn], rec[64:128, :tn], ALU.mult), [rrec], [rops, ryo[b]])
    P.dma("sp", g.Ymine[yrow0:yrow0 + 64, t0:t0 + tn], yo[b][:, :tn], reads=[ryo[b]], writes=[g.rY])


def na_head(g, l, h):
    C, P, A = g.C, g.P, g.A
    if True:
        A.reset()
        g.rqk = Res()
        scr = attn_scratch(g, False)
        qT = A.bf16([64, N]); kT = A.bf16([64, N]); vaug = A.bf16([128, 66, 128]); rv = Res()
        tab = A.f32([128, 3, 8, 512]); rtab = Res()
        lg = [A.f32([128, 512]) for _ in range(2)]; rlg = [Res(), Res()]
        pb = [A.bf16([128, 512]) for _ in range(3)]; rpbuf = [Res() for _ in range(3)]
        fin = (A.f32([128, 512]), Res(), [A.f32([64, 512]) for _ in range(2)], [Res(), Res()], [0])
        for v in range(3):
            for k2 in range(2):
                C.ld(tab[:, v, 4 * k2:4 * k2 + 4, :], g.natab[l][h, v, :, 4 * k2:4 * k2 + 4, :], rtab)
        prep_qk(g, 0 + 64 * h, 0, qT, False, scr)
        prep_qk(g, 128 + 64 * h, 1, kT, False, scr)
        prep_v(g, 64 * h, vaug, rv)
        ps_s = g.ps[2:5]; rps_s = g.rps[2:5]
        ps_o = g.ps[5:7]; rps_o = g.rps[5:7]
        si = 0
        for qi, (t0, tn) in enumerate(QT):
            o_ps, rops = ps_o[qi % 2], rps_o[qi % 2]
            keys = [(0, None), (1, None)]
            if qi > 0:
                qt = qi - 1
                var = 0 if qt == 0 else (2 if qt == 15 else 1)
                for kr in range(8):
                    kg = 4 * qt - 2 + kr
                    if 0 <= kg <= 63:
                        keys.append((2 + kg, (var, kr)))
            for ki, (ktile, slot) in enumerate(keys):
                sb_ = si % 3
                si += 1
                P.mm(ps_s[sb_][:, :tn], kT[:, 128 * ktile:128 * ktile + 128], qT[:, t0:t0 + tn], True, True, [g.rqk], [rps_s[sb_]])
                if slot is None:
                    P.op("act", lambda e, sb_=sb_, tn=tn: e.activation(pb[sb_][:, :tn], ps_s[sb_][:, :tn], AF.Exp, scale=0.125), [], [rps_s[sb_], rpbuf[sb_]])
                else:
                    lb_ = si % 2
                    P.op("dve", lambda e, sb_=sb_, lb_=lb_, slot=slot, tn=tn: e.scalar_tensor_tensor(lg[lb_][:, :tn], ps_s[sb_][:, :tn], 0.125, tab[:, slot[0], slot[1], :tn], ALU.mult, ALU.add),
                         [rtab], [rps_s[sb_], rlg[lb_]])
                    P.op("act", lambda e, sb_=sb_, lb_=lb_, tn=tn: e.activation(pb[sb_][:, :tn], lg[lb_][:, :tn], AF.Exp), [rlg[lb_]], [rpbuf[sb_]])
                P.mm(o_ps[:, :tn], vaug[:, ktile, :], pb[sb_][:, :tn], ki == 0, ki == len(keys) - 1, [rv, rpbuf[sb_]], [rops])
            if getattr(g, "dbgna", False) and h == 0 and qi == 0:
                d1 = A.f32([128, 4, 256]); rd1 = Res()
                P.op("dve", lambda e: e.tensor_copy(d1[0:64, 0, :], qT[:, 0:256]), [g.rqk], [rd1])
                P.op("dve", lambda e: e.tensor_copy(d1[0:64, 1, :], kT[:, 0:256]), [g.rqk], [rd1])
                P.op("dve", lambda e, sb_=sb_: e.tensor_copy(d1[:, 2, :], pb[sb_][:, 0:256]), [rpbuf[sb_]], [rd1])
                P.op("dve", lambda e: e.tensor_copy(d1[:, 3, :], o_ps[:, 0:256]), [], [rops, rd1])
                P.dma("sp", g.dbg_na, d1, reads=[rd1], writes=[g.rY])
                d2 = A.f32([128, 256]); rd2 = Res()
                P.op("dve", lambda e: e.tensor_copy(d2[:, 0:128], vaug[:, 1, :]), [rv], [rd2])
                P.op("dve", lambda e: e.tensor_copy(d2[0:64, 128:256], scr[4][:, 0:128]), [scr[5]], [rd2])
                P.dma("sp", g.dbg_na2, d2, reads=[rd2], writes=[g.rY])
            finalize_attn(g, o_ps, rops, tn, 64 * h, t0, None, fin)
            if getattr(g, "dbgna", False) and qi == 0:
                d3 = lg[0].rearrange("p (a b) -> p a b", a=2); rd3 = rlg[0]
                P.op("dve", lambda e: e.tensor_copy(d3[64:128, 0, :], fin[0][64:128, 0:256]), [fin[1]], [rd3])
                P.op("dve", lambda e: e.tensor_copy(d3[0:64, 1, :], fin[2][0][:, 0:256]), [fin[3][0]], [rd3])
                P.op("dve", lambda e: e.tensor_copy(d3[0:64, 0, :], fin[2][0][:, 0:256]), [fin[3][0]], [rd3])
                P.op("dve", lambda e: e.tensor_copy(d3[64:128, 1, :], fin[0][64:128, 0:256]), [fin[1]], [rd3])
                P.dma("sp", g.dbg_na3, d3, reads=[rd3], writes=[g.rY])
                r9 = Res()
                P.dma("sp", g.dbg_na4, g.Ymine[0:64, 0:256], reads=[g.rY], writes=[r9]); C.outs.append(r9)
                break
        barrier(P)
        if getattr(g, "dbgna", False):
            return


def wa_heads(g, l):
    C, P, A = g.C, g.P, g.A
    A.reset()
    g.rqk = Res()
    scr = attn_scratch(g)
    kT = A.bf16([64, N]); vaug = A.bf16([128, 66, 128]); rv = Res()
    qT = A.bf16([64, N])
    wm = A.bf16([128, 2, 128]); rwm = Res()
    pb = [A.bf16([128, 512]) for _ in range(3)]; rpbuf = [Res() for _ in range(3)]
    fin = (A.f32([128, 512]), Res(), [A.f32([64, 512]) for _ in range(2)], [Res(), Res()], [0])
    P.dma("pool", wm, g.wamask, writes=[rwm])
    prep_qk(g, 896, 3, kT, True, scr)
    prep_v(g, 512, vaug, rv)
    ps_s = g.ps[2:5]; rps_s = g.rps[2:5]
    ps_o = g.ps[5:7]; rps_o = g.rps[5:7]
    si = 0
    for h in range(2):
        prep_qk(g, 768 + 64 * h, 2, qT, True, scr)
        for qi, (t0, tn) in enumerate(QT):
            o_ps, rops = ps_o[qi % 2], rps_o[qi % 2]
            keys = [(0, 0, tn, ()), (1, 0, tn, ())]
            if qi > 0:
                qt = qi - 1
                for kb in range(4 * qt - 1, 4 * qt + 5):
                    if 0 <= kb <= 63:
                        lo = max(4 * qt, kb - 1); hi = min(4 * qt + 3, kb + 1)
                        masks = []
                        for nb in range(lo, hi + 1):
                            if nb == kb + 1:
                                masks.append(((nb - 4 * qt) * 128, 0))
                            elif nb == kb - 1:
                                masks.append(((nb - 4 * qt) * 128, 1))
                        keys.append((2 + kb, (lo - 4 * qt) * 128, (hi - 4 * qt + 1) * 128, tuple(masks)))
            for ki, (ktile, c0, c1, masks) in enumerate(keys):
                sb_ = si % 3
                si += 1
                P.mm(ps_s[sb_][:, c0:c1], kT[:, 128 * ktile:128 * ktile + 128], qT[:, t0 + c0:t0 + c1], True, True, [g.rqk], [rps_s[sb_]])
                P.op("act", lambda e, sb_=sb_, c0=c0, c1=c1: e.activation(pb[sb_][:, c0:c1], ps_s[sb_][:, c0:c1], AF.Exp, scale=0.125), [], [rps_s[sb_], rpbuf[sb_]])
                for (mc, which) in masks:
                    P.op("pool", lambda e, sb_=sb_, mc=mc, which=which: e.tensor_tensor(pb[sb_][:, mc:mc + 128], pb[sb_][:, mc:mc + 128], wm[:, which, :], ALU.mult), [rwm], [rpbuf[sb_]])
                P.mm(o_ps[:, c0:c1], vaug[:, ktile, :], pb[sb_][:, c0:c1], ki == 0, ki == len(keys) - 1, [rv, rpbuf[sb_]], [rops], skip_group_check=True)
            finalize_attn(g, o_ps, rops, tn, 128 + 64 * h, t0, h, fin)
    barrier(P)


def phase_s2_attn(g, l):
    for h in range(2):
        na_head(g, l, h)
        if getattr(g, "dbgna", False):
            return
    wa_heads(g, l)


def phase_s2_hgrn(g, l):
    C, P, A = g.C, g.P, g.A
    A.reset()
    osum = A.f32([128, N]); rosum = Res()
    Sf = A.f32([128, 128]); Sb = A.bf16([128, 128]); rS = Res()
    tri = A.f32([64, 6, 64]); rtri = Res()
    C.ld(tri, g.tri, rtri, "sp")
    lbt = A.f32([128, 2, 2]); lrow = A.f32([64, 2, 2, 128]); rlb = Res()
    if l == 0:
        for d in range(2):
            P.op("pool", lambda e, d=d: e.memset(lbt[:, d, 0:1], 0.0), [], [rlb])
            P.op("pool", lambda e, d=d: e.memset(lbt[:, d, 1:2], 1.0), [], [rlb])
            P.op("pool", lambda e, d=d: e.memset(lrow[:, d, 0, :], 0.0), [], [rlb])
            P.op("pool", lambda e, d=d: e.memset(lrow[:, d, 1, :], 1.0), [], [rlb])
    else:
        hl = A.f32([128, 2, 2]); hr = A.f32([64, 2, 2, 128]); rh_ = Res()
        C.ld(hl, g.hglow, rh_, "sp"); C.ld(hr, g.hglow_row, rh_, "sp")
        for d in range(2):
            P.op("dve", lambda e, d=d: e.tensor_tensor(lbt[:, d, 0:1], hl[:, d, 1:2], hl[:, d, 0:1], ALU.subtract), [rh_], [rlb])
            P.op("act", lambda e, d=d: e.activation(lbt[:, d, 0:1], lbt[:, d, 0:1], AF.Sigmoid), [], [rlb])
            P.op("dve", lambda e, d=d: e.tensor_scalar(lbt[:, d, 1:2], lbt[:, d, 0:1], -1.0, 1.0, ALU.mult, ALU.add), [], [rlb])
            P.op("dve", lambda e, d=d: e.tensor_tensor(lrow[:, d, 0, :], hr[:, d, 1, :], hr[:, d, 0, :], ALU.subtract), [rh_], [rlb])
            P.op("act", lambda e, d=d: e.activation(lrow[:, d, 0, :], lrow[:, d, 0, :], AF.Sigmoid), [], [rlb])
            P.op("dve", lambda e, d=d: e.tensor_scalar(lrow[:, d, 1, :], lrow[:, d, 0, :], -1.0, 1.0, ALU.mult, ALU.add), [], [rlb])
    W = 512
    qf = [A.f32([128, W]) for _ in range(2)]; ff = [A.f32([128, W]) for _ in range(2)]; rin = [Res(), Res()]
    ftm = [A.f32([64, 8, 128]) for _ in range(2)]; vbf = [A.bf16([64, 8, 128]) for _ in range(2)]
    fm_f = A.f32([128, W]); kf = A.f32([128, W]); qs = A.f32([128, W]); b_sb = A.f32([128, W]); ex = A.f32([128, W])
    qtil = A.bf16([128, W]); ktil = A.bf16([128, W]); qhat = A.bf16([128, W])
    ft = A.f32([64, 8, 128]); lgt = A.f32([64, 8, 128]); kt_ = A.f32([64, 8, 128]); eR = A.f32([64, 8, 128]); khat = A.bf16([64, 8, 128])
    attT = A.bf16([64, 8, 64]); sc_ = A.f32([128, 3, 8])
    attf = A.f32([64, 8, 64]); ex2 = A.f32([128, W]); ex3 = A.f32([128, W]); qx = A.bf16([128, W]); kx = A.bf16([128, W])
    ind = A.f32([128, 2, 2, 64])
    C.ld(ind, g.hind, rtri, "sp")
    rblk = Res()
    ps_b, rpb_ = g.ps[0], g.rps[0]
    ps_R = g.ps[1:3]; rpR = g.rps[1:3]
    ps_att, rpatt = g.ps[3], g.rps[3]
    ps_o, rpo = g.ps[4], g.rps[4]
    ps_st = g.ps[5:7]; rpst = g.rps[5:7]
    ps_ax, rpax = g.ps[7], g.rps[7]
    blocks = [(0, 4)] + [(256 + 512 * i, 8) for i in range(16)]
    sti = 0
    bi_ = 0
    for d in range(2):
        P.op("pool", lambda e: e.memset(Sf, 0.0), [], [rS])
        P.op("pool", lambda e: e.memset(Sb, 0.0), [], [rS])
        order = blocks if d == 0 else [blocks[0]] + blocks[:0:-1]
        frow = 384 + 128 * d
        fcol = 256 + 128 * d
        for (t0, nb) in order:
            b = bi_ % 2
            bi_ += 1
            Wb = 64 * nb
            C.ld(qf[b][:, :Wb], g.S2in_fm[256:384, t0:t0 + Wb], rin[b], "sp")
            C.ld(ff[b][:, :Wb], g.S2in_fm[frow:frow + 128, t0:t0 + Wb], rin[b], "sp")
            C.ld(ftm[b][:, :nb, :], g.S2in_tm[t0:t0 + Wb, fcol:fcol + 128].rearrange("(n p) c -> p n c", p=64), rin[b], "sp")
            P.dma("pool", vbf[b][:, :nb, :], g.S2in_tm[t0:t0 + Wb, 128:256].rearrange("(n p) c -> p n c", p=64), writes=[rin[b]])
            P.op("act", lambda e, b=b, Wb=Wb: e.activation(fm_f[:, :Wb], ff[b][:, :Wb], AF.Sigmoid), [rin[b]], [rblk])
            P.op("dve", lambda e, d=d, Wb=Wb: e.tensor_scalar(fm_f[:, :Wb], fm_f[:, :Wb], lbt[:, d, 1:2], lbt[:, d, 0:1], ALU.mult, ALU.add), [rlb], [rblk])
            P.op("dve", lambda e, Wb=Wb: e.tensor_scalar(kf[:, :Wb], fm_f[:, :Wb], -1.0, 1.0, ALU.mult, ALU.add), [], [rblk])
            P.op("act", lambda e, b=b, Wb=Wb: e.activation(qs[:, :Wb], qf[b][:, :Wb], AF.Silu), [rin[b]], [rblk])
            P.op("act", lambda e, b=b, nb=nb: e.activation(ft[:, :nb, :], ftm[b][:, :nb, :], AF.Sigmoid), [rin[b]], [rblk])
            P.op("dve", lambda e, d=d, nb=nb: e.tensor_tensor(ft[:, :nb, :], ft[:, :nb, :], lrow[:, d, 1, :].unsqueeze(1).broadcast_to([64, nb, 128]), ALU.mult), [rlb], [rblk])
            P.op("dve", lambda e, d=d, nb=nb: e.tensor_tensor(ft[:, :nb, :], ft[:, :nb, :], lrow[:, d, 0, :].unsqueeze(1).broadcast_to([64, nb, 128]), ALU.add), [rlb], [rblk])
            P.op("dve", lambda e, nb=nb: e.tensor_scalar(kt_[:, :nb, :], ft[:, :nb, :], -1.0, 1.0, ALU.mult, ALU.add), [], [rblk])
            P.op("dve", lambda e, nb=nb: e.tensor_scalar(ft[:, :nb, :], ft[:, :nb, :], 1e-30, None, ALU.max), [], [rblk])
            P.op("act", lambda e, nb=nb: e.activation(lgt[:, :nb, :], ft[:, :nb, :], AF.Ln), [], [rblk])
            for c in range(nb):
                P.mm(ps_b[:, 64 * c:64 * c + 64], lgt[:, c, :], tri[:, d, :], c == 0, True, [rblk, rtri], [rpb_], skip_group_check=True)
            for c in range(nb):
                P.mm(ps_R[c // 4][0:64, 128 * (c % 4):128 * (c % 4) + 128], tri[:, 2 + d, :], lgt[:, c, :], c % 4 == 0, True, [rblk, rtri], [rpR[c // 4]], skip_group_check=True)
            P.op("act", lambda e, Wb=Wb: e.copy(b_sb[:, :Wb], ps_b[:, :Wb]), [], [rpb_, rblk])
            for hf in range((nb + 3) // 4):
                n4 = min(4, nb - 4 * hf)
                P.op("act", lambda e, hf=hf, n4=n4: e.activation(eR[:, 4 * hf:4 * hf + n4, :], ps_R[hf][0:64, 0:128 * n4].rearrange("p (a b) -> p a b", a=n4), AF.Exp), [], [rpR[hf], rblk])
            P.op("dve", lambda e, nb=nb: e.tensor_tensor(khat[:, :nb, :], kt_[:, :nb, :], eR[:, :nb, :], ALU.mult), [], [rblk])
            bv = b_sb[:, :Wb].rearrange("p (c t) -> p c t", t=64)
            last = 63 if d == 0 else 0
            rA, rB, rbd = (15, 47, 31) if d == 0 else (48, 16, 32)
            hA, hB = ((0, 32), (32, 64)) if d == 0 else ((32, 64), (0, 32))
            P.op("act", lambda e, nb=nb, bv=bv, last=last: e.activation(sc_[:, 2, :nb], bv[:, :, last], AF.Exp), [], [rblk])
            for c in range(nb):
                for (rc_, (h0, h1)) in ((rA, hA), (rB, hB)):
                    P.op("dve", lambda e, c=c, rc_=rc_, h0=h0, h1=h1: e.tensor_scalar(ex[:, 64 * c + h0:64 * c + h1], b_sb[:, 64 * c + h0:64 * c + h1], b_sb[:, 64 * c + rc_:64 * c + rc_ + 1], None, ALU.subtract), [], [rblk])
                P.op("dve", lambda e, c=c, rbd=rbd: e.tensor_scalar(ex2[:, 64 * c:64 * c + 64], b_sb[:, 64 * c:64 * c + 64], b_sb[:, 64 * c + rbd:64 * c + rbd + 1], None, ALU.subtract), [], [rblk])
            P.op("act", lambda e, Wb=Wb: e.activation(ex3[:, :Wb], ex[:, :Wb], AF.Exp), [], [rblk])
            P.op("dve", lambda e, Wb=Wb: e.tensor_tensor(qtil[:, :Wb], qs[:, :Wb], ex3[:, :Wb], ALU.mult), [], [rblk])
            P.op("act", lambda e, Wb=Wb: e.activation(ex3[:, :Wb], ex[:, :Wb], AF.Exp, scale=-1.0), [], [rblk])
            P.op("dve", lambda e, Wb=Wb: e.tensor_tensor(ktil[:, :Wb], kf[:, :Wb], ex3[:, :Wb], ALU.mult), [], [rblk])
            P.op("dve", lambda e, Wb=Wb: e.tensor_scalar(ex[:, :Wb], ex2[:, :Wb], 0.0, None, ALU.min), [], [rblk])
            P.op("act", lambda e, Wb=Wb: e.activation(ex3[:, :Wb], ex[:, :Wb], AF.Exp), [], [rblk])
            P.op("dve", lambda e, Wb=Wb: e.tensor_tensor(ex3[:, :Wb], ex3[:, :Wb], qs[:, :Wb], ALU.mult), [], [rblk])
            P.op("dve", lambda e, nb=nb, d=d: e.tensor_tensor(qx[:, :64 * nb].rearrange("p (c t) -> p c t", t=64), ex3[:, :64 * nb].rearrange("p (c t) -> p c t", t=64),
                                                             ind[:, d, 1, :].unsqueeze(1).broadcast_to([128, nb, 64]), ALU.mult), [rtri], [rblk])
            P.op("dve", lambda e, Wb=Wb: e.tensor_scalar(ex[:, :Wb], ex2[:, :Wb], 0.0, None, ALU.max), [], [rblk])
            P.op("act", lambda e, Wb=Wb: e.activation(ex3[:, :Wb], ex[:, :Wb], AF.Exp, scale=-1.0), [], [rblk])
            P.op("dve", lambda e, Wb=Wb: e.tensor_tensor(ex3[:, :Wb], ex3[:, :Wb], kf[:, :Wb], ALU.mult), [], [rblk])
            P.op("dve", lambda e, nb=nb, d=d: e.tensor_tensor(kx[:, :64 * nb].rearrange("p (c t) -> p c t", t=64), ex3[:, :64 * nb].rearrange("p (c t) -> p c t", t=64),
                                                             ind[:, d, 0, :].unsqueeze(1).broadcast_to([128, nb, 64]), ALU.mult), [rtri], [rblk])
            P.op("act", lambda e, Wb=Wb: e.activation(ex[:, :Wb], b_sb[:, :Wb], AF.Exp), [], [rblk])
            P.op("dve", lambda e, Wb=Wb: e.tensor_tensor(qhat[:, :Wb], qs[:, :Wb], ex[:, :Wb], ALU.mult), [], [rblk])
            for c in range(nb):
                P.mm(ps_att[0:64, 64 * c:64 * c + 64], ktil[:, 64 * c:64 * c + 64], qtil[:, 64 * c:64 * c + 64], c == 0, True, [rblk], [rpatt], skip_group_check=True)
            for c in range(nb):
                P.mm(ps_ax[0:64, 64 * c:64 * c + 64], kx[:, 64 * c:64 * c + 64], qx[:, 64 * c:64 * c + 64], c == 0, True, [rblk], [rpax], skip_group_check=True)
            P.op("dve", lambda e, nb=nb, d=d: e.tensor_tensor(attf[:, :nb, :], ps_att[0:64, 0:64 * nb].rearrange("p (a b) -> p a b", a=nb),
                                                             tri[:, 4 + d, :].unsqueeze(1).broadcast_to([64, nb, 64]), ALU.mult), [rtri], [rpatt, rblk])
            P.op("dve", lambda e, nb=nb: e.tensor_tensor(attT[:, :nb, :], ps_ax[0:64, 0:64 * nb].rearrange("p (a b) -> p a b", a=nb), attf[:, :nb, :], ALU.add), [], [rpax, rblk])
            corder = range(nb) if d == 0 else range(nb - 1, -1, -1)
            for ci, c in enumerate(corder):
                P.mm(ps_o[:, 64 * c:64 * c + 64], vbf[b][:, c, :], attT[:, c, :], ci == 0, False, [rblk, rin[b]], [rpo], skip_group_check=True)
                P.mm(ps_o[:, 64 * c:64 * c + 64], Sb, qhat[:, 64 * c:64 * c + 64], False, True, [rS, rblk], [rpo], skip_group_check=True)
                sb_ = sti % 2
                sti += 1
                P.mm(ps_st[sb_][:, 0:128], khat[:, c, :], vbf[b][:, c, :], True, True, [rblk, rin[b]], [rpst[sb_]])
                P.op("dve", lambda e, c=c, sb_=sb_: e.scalar_tensor_tensor(Sf, Sf, sc_[:, 2, c:c + 1], ps_st[sb_][:, 0:128], ALU.mult, ALU.add), [rblk], [rpst[sb_], rS])
                P.op("act", lambda e: e.copy(Sb, Sf), [], [rS])
            if d == 0:
                P.op("act", lambda e, t0=t0, Wb=Wb: e.copy(osum[:, t0:t0 + Wb], ps_o[:, :Wb]), [], [rpo, rosum])
            else:
                P.op("dve", lambda e, t0=t0, Wb=Wb: e.tensor_tensor(osum[:, t0:t0 + Wb], ps_o[:, :Wb], osum[:, t0:t0 + Wb], ALU.add), [], [rpo, rosum])
    gt = [qf[0], qf[1]]; rg = rin
    yo = [ff[0], ff[1]]; ryo = rin
    for ti, (t0, tn) in enumerate(QT):
        b = ti % 2
        C.ld(gt[b][:, :tn], g.S2in_fm[640:768, t0:t0 + tn], rg[b], "sp")
        P.op("act", lambda e, t0=t0, tn=tn: e.activation(fm_f[:, :tn], osum[:, t0:t0 + tn], AF.Square), [rosum], [rblk])
        P.mm(ps_b[:, :tn], g.ones, fm_f[:, :tn], True, True, [rblk, g.rconst], [rpb_])
        rsqrt_ps(g, kf[:, :tn], ps_b[:, :tn], 1.0 / 128, [], [rpb_], rblk)
        P.op("dve", lambda e, t0=t0, tn=tn: e.scalar_tensor_tensor(qs[:, :tn], osum[:, t0:t0 + tn], g.pvec[:, 4:5], kf[:, :tn], ALU.mult, ALU.mult), [rosum, g.rpv], [rblk])
        P.op("act", lambda e, b=b, tn=tn: e.activation(gt[b][:, :tn], gt[b][:, :tn], AF.Silu), [], [rg[b]])
        P.op("dve", lambda e, b=b, tn=tn: e.tensor_tensor(yo[b][:, :tn], qs[:, :tn], gt[b][:, :tn], ALU.mult), [rg[b], rblk], [ryo[b]])
        P.dma("sp", g.Ymine[256:384, t0:t0 + tn], yo[b][:, :tn], reads=[ryo[b]], writes=[g.rY])
    barrier(P)


def phase_s2(g, l):
    C, P = g.C, g.P
    C.ld(g.pvec, g.pvec_d[l], g.rpv, "sp")
    C.ld(g.esink, g.wsink_d[l], g.rpv, "sp")
    P.op("act", lambda e: e.activation(g.esink, g.esink, AF.Exp), [], [g.rpv])
    C.ld(g.ropeR, g.ropeR_d, g.rpv, "sp")
    P.op("pool", lambda e: e.memset(g.zcol, 0.0), [], [g.rpv])
    phase_s2_attn(g, l)
    if getattr(g, "dbgna", False):
        return
    phase_s2_hgrn(g, l)


def s2_host_inputs(inp, core, layers):
    b, j = core // 4, core % 4
    m = {}
    T = 8192
    pos = np.arange(T)
    rowp = (pos // 64).astype(np.float32); colp = (pos % 64).astype(np.float32)
    inv = (np.float32(10000.0) ** (-(np.arange(16, dtype=np.float32)) / np.float32(16))).astype(np.float32)
    Cc = np.ones((64, N), np.float32); Ss = np.zeros((64, N), np.float32)
    for dd in range(64):
        p_ = rowp if dd < 32 else colp
        ang = (p_ * inv[dd % 16]).astype(np.float32)
        Cc[dd, L:] = np.cos(ang); Ss[dd, L:] = np.sin(ang)
    m["ropeC"] = Cc; m["ropeS"] = Ss
    R = np.zeros((64, 64), np.float32)
    for mm_ in range(64):
        if mm_ % 32 < 16:
            R[mm_, mm_ + 16] = -1.0
        else:
            R[mm_, mm_ - 16] = 1.0
    m["ropeR"] = _c(R.T)
    kk = np.arange(128)[:, None]; qq = np.arange(128)[None, :]
    m["wamask"] = _c(np.stack([(kk >= qq), (kk <= qq)], 1).astype(np.float32))
    jj = np.arange(64)[:, None]; ii = np.arange(64)[None, :]
    same = (jj // 32) == (ii // 32)
    m["tri"] = _c(np.stack([(jj <= ii), (jj >= ii), (jj > ii), (jj < ii), (jj <= ii) & same, (jj >= ii) & same], 1).astype(np.float32))
    tok = np.arange(64)
    ind = np.zeros((128, 2, 2, 64), np.float32)
    ind[:, 0, 0, :] = tok < 32; ind[:, 0, 1, :] = tok >= 32
    ind[:, 1, 0, :] = tok >= 32; ind[:, 1, 1, :] = tok < 32
    m["hind"] = ind
    for l in layers:
        pv = np.zeros((128, 5), np.float32)
        pv[:64, 0] = inp["na_q_norm"][l]; pv[:64, 1] = inp["na_k_norm"][l]; pv[:64, 2] = inp["wa_q_norm"][l]; pv[:64, 3] = inp["wa_k_norm"][l]
        pv[:, 4] = inp["hg_norm"][l]
        m["pvec%d" % l] = pv
        m["wsink%d" % l] = _c(np.tile(np.asarray(inp["wa_sink"][l])[None, 2 * j:2 * j + 2], (128, 1)))
        rpb = np.asarray(inp["na_rpb"][l], np.float32)
        tab = np.full((2, 3, 128, 8, 512), -30000.0, np.float32)
        kkk = np.arange(128); q = np.arange(512)
        for v, qt in enumerate((0, 1, 15)):
            rq = (8 * qt + q // 64)[None, :]; cq = (q % 64)[None, :]
            rs = np.clip(rq - 4, 0, 120); cs_ = np.clip(cq - 8, 0, 48)
            for kr in range(8):
                kg = 4 * qt - 2 + kr
                if not (0 <= kg <= 63):
                    continue
                rk = (2 * kg + kkk // 64)[:, None]; ck = (kkk % 64)[:, None]
                ok = (rk >= rs) & (rk < rs + 8) & (ck >= cs_) & (ck < cs_ + 16)
                dr = np.clip(rk - rq + 7, 0, 14); dc = np.clip(ck - cq + 15, 0, 30)
                for hh in range(2):
                    tab[hh, v, :, kr, :] = np.where(ok, rpb[2 * j + hh][dr, dc], np.float32(-30000.0))
        m["natab%d" % l] = tab
    hl = np.asarray(inp["hg_lower"], np.float32)[:, :, 128 * j:128 * j + 128]
    m["hglow"] = _c(hl.transpose(2, 0, 1))
    m["hglow_row"] = _c(np.tile(hl[None], (64, 1, 1, 1)))
    return m


def phase_x2(g):
    P = g.P
    rg = Res()
    P.op("pool", lambda e: e.collective_compute("AllGather", ALU.bypass, replica_groups=ALLR, ins=[g.Ymine], outs=[g.Yg]), [g.rY], [rg], dma="cc")
    barrier(P)
    items = []
    for t, trow in ((0, 0), (2, 128), (1, 256)):
        for r in range(4):
            sc_, sl_ = [], []
            for s8 in range(8):
                bb, jj = s8 // 4, s8 % 4
                r0 = (4 * bb + r) * 384 + trow
                sc_.append(g.Yg[r0:r0 + 128, 64 * jj:64 * jj + 64])
                sl_.append(g.Yg[r0:r0 + 128, L + 2048 * jj:L + 2048 * jj + 2048])
            tt = {0: 0, 256: 1, 128: 2}[trow]
            d0 = (tt * 4 + r) * 128
            items.append((sc_, g.S3in_y[d0:d0 + 128, 0:64], [128, 64]))
            items.append((sl_, g.S3in_y[d0:d0 + 128, 64:NT], [128, 2048]))
    select4(g, items)


def phase_s3(g, l):
    C, P, A = g.C, g.P, g.A
    A.reset()
    scr = norm_scratch(g)
    hT = A.bf16([128, 8, 512]); rh = Res()
    yT = A.bf16([128, 12, 512]); ry = Res()
    wg = [A.bf16([128, 8, 128]) for _ in range(2)]; rwg = [Res(), Res()]
    wp = [A.bf16([128, 4, 128]) for _ in range(2)]; rwp = [Res(), Res()]
    wo = [A.bf16([128, 8, 128]) for _ in range(2)]; rwo = [Res(), Res()]
    sig = A.f32([128, 512]); rsig = Res()
    macc = A.f32([128, 512]); rmacc = Res()
    tmp = A.f32([128, 512]); rtmp = Res()
    merged = A.bf16([128, 8, 512]); rmg = Res()
    Wg_v = g.Wg[l].rearrange("(k p) n -> p k n", p=128)
    wps = [g.wpa[l].rearrange("(k p) n -> p k n", p=128), g.wpb[l].rearrange("(k p) n -> p k n", p=128), g.wpc[l].rearrange("(k p) n -> p k n", p=128)]
    wo_v = g.wout[l].rearrange("(k p) n -> p k n", p=128)
    yv = g.S3in_y.rearrange("(k p) t -> p k t", p=128)
    pg = g.ps[2:4]; rpg = g.rps[2:4]
    pp_ = g.ps[4:6]; rpp_ = g.rps[4:6]
    pm = g.ps[6:8]; rpm = g.rps[6:8]
    wi = 0
    for ti, (t0, tn, c) in enumerate(TILES):
        norm_mod_tile(g, t0, tn, g.A1, g.B1, c, hT[:, :, :tn], rh, scr)
        P.dma("pool", yT[:, 0:6, :tn], yv[:, 0:6, t0:t0 + tn], writes=[ry])
        P.dma("pool", yT[:, 6:12, :tn], yv[:, 6:12, t0:t0 + tn], writes=[ry])
        for fc in range(8):
            for br in range(3):
                b = wi % 2
                wi += 1
                P.dma("pool", wg[b], Wg_v[:, :, br * 1024 + fc * 128:br * 1024 + fc * 128 + 128], writes=[rwg[b]])
                P.dma("pool", wp[b], wps[br][:, :, fc * 128:fc * 128 + 128], writes=[rwp[b]])
                for k in range(8):
                    P.mm(pg[b][:, :tn], wg[b][:, k, :], hT[:, k, :tn], k == 0, k == 7, [rwg[b], rh], [rpg[b]])
                P.op("act", lambda e, b=b, tn=tn: e.activation(sig[:, :tn], pg[b][:, :tn], AF.Sigmoid), [], [rpg[b], rsig])
                for kk in range(4):
                    P.mm(pp_[b][:, :tn], wp[b][:, kk, :], yT[:, 4 * br + kk, :tn], kk == 0, kk == 3, [rwp[b], ry], [rpp_[b]])
                if br == 0:
                    P.op("dve", lambda e, b=b, tn=tn: e.tensor_tensor(macc[:, :tn], pp_[b][:, :tn], sig[:, :tn], ALU.mult), [rsig], [rpp_[b], rmacc])
                else:
                    P.op("dve", lambda e, b=b, tn=tn: e.tensor_tensor(tmp[:, :tn], pp_[b][:, :tn], sig[:, :tn], ALU.mult), [rsig], [rpp_[b], rtmp])
                    P.op("pool", lambda e, tn=tn: e.tensor_tensor(macc[:, :tn], macc[:, :tn], tmp[:, :tn], ALU.add), [rtmp], [rmacc])
            P.op("act", lambda e, fc=fc, tn=tn: e.copy(merged[:, fc, :tn], macc[:, :tn]), [rmacc], [rmg])
        for oc in range(8):
            b = oc % 2
            P.dma("pool", wo[b], wo_v[:, :, oc * 128:oc * 128 + 128], writes=[rwo[b]])
            for k in range(8):
                P.mm(pm[b][:, :tn], wo[b][:, k, :], merged[:, k, :tn], k == 0, k == 7, [rwo[b], rmg], [rpm[b]])
            P.op("dve", lambda e, b=b, oc=oc, t0=t0, tn=tn, c=c: e.scalar_tensor_tensor(g.x_sb[:, oc, t0:t0 + tn], pm[b][:, :tn], g.mod[:, 16 + oc, c:c + 1], g.x_sb[:, oc, t0:t0 + tn], ALU.mult, ALU.add),
                 [g.rmod], [rpm[b], g.rx])
    barrier(P)
    A.reset()
    h2 = A.bf16([128, 8, NT]); rh2 = Res()
    wT = A.f32([16, NT]); rwT = Res()
    mark = A.off
    scr = norm_scratch(g)
    h2f = A.f32([128, 8, 512])
    NS = 17
    lgts = A.f32([128, NS, 16]); prob = A.f32([128, NS, 16]); sel = A.f32([128, NS, 16]); msk = A.f32([128, NS, 16])
    pmn = A.f32([128, NS, 4, 6]); t1 = A.f32([128, NS, 4]); t2 = A.f32([128, NS, 4]); gs = A.f32([128, NS, 4])
    v1 = A.f32([128, NS]); v2 = A.f32([128, NS])
    wr = A.f32([128, 8, 16]); brt = A.f32([128, 16]); ident = A.f32([128, 128]); rr_ = Res()
    C.ld(wr, g.wrouter.rearrange("(k p) e -> p k e", p=128), rr_, "sp")
    C.ld(brt, g.brouter, rr_, "sp")
    C.ld(ident, g.ident, rr_, "sp")
    rrt = Res()
    ps_l, rpl = g.ps[2], g.rps[2]
    subs = [(0, 64)] + [(64 + 128 * i, 128) for i in range(16)]
    P.op("dve", lambda e: e.memset(ps_l[:, 0:16 * NS], 0.0), [], [rpl])
    for ti, (t0, tn, c) in enumerate(TILES):
        norm_mod_tile(g, t0, tn, g.A2, g.B2, c, h2[:, :, t0:t0 + tn], rh2, scr, hf_out=h2f)
        for si, (s0, sn) in enumerate(subs):
            if not (t0 <= s0 < t0 + tn):
                continue
            for k in range(8):
                P.mm(ps_l[0:sn, 16 * si:16 * si + 16], h2f[:, k, s0 - t0:s0 - t0 + sn], wr[:, k, :], False, k == 7, [rh2, rr_], [rpl], skip_group_check=True)
    P.op("pool", lambda e: e.memset(lgts[:, 0, :], 0.0), [], [rrt])
    P.op("dve", lambda e: e.tensor_copy(lgts[0:64, 0, :], ps_l[0:64, 0:16]), [], [rpl, rrt])
    P.op("dve", lambda e: e.tensor_copy(lgts[:, 1:NS, :], ps_l[:, 16:16 * NS].rearrange("p (a b) -> p a b", b=16)), [], [rpl, rrt])

    def dv(fn, extra=()):
        P.op("dve", fn, list(extra), [rrt])
    bcast = lambda ap, shape, ax: ap.unsqueeze(ax).broadcast_to(list(shape))
    dv(lambda e: e.tensor_reduce(v1, lgts, AX.X, ALU.max))
    dv(lambda e: e.tensor_tensor(prob, lgts, bcast(v1, [128, NS, 16], 2), ALU.subtract))
    P.op("act", lambda e: e.activation(prob, prob, AF.Exp), [], [rrt])
    dv(lambda e: e.tensor_reduce(v2, prob, AX.X, ALU.add))
    dv(lambda e: e.reciprocal(v2, v2))
    dv(lambda e: e.tensor_tensor(prob, prob, bcast(v2, [128, NS, 16], 2), ALU.mult))
    dv(lambda e: e.tensor_tensor(sel, prob, bcast(brt, [128, NS, 16], 1), ALU.add), [rr_])
    s4 = sel.rearrange("p a (g e) -> p a g e", e=4)
    dv(lambda e: e.tensor_tensor(pmn[:, :, :, 0:3], s4[:, :, :, 0:3], s4[:, :, :, 1:4], ALU.min))
    dv(lambda e: e.tensor_tensor(pmn[:, :, :, 3:5], s4[:, :, :, 0:2], s4[:, :, :, 2:4], ALU.min))
    dv(lambda e: e.tensor_tensor(pmn[:, :, :, 5:6], s4[:, :, :, 0:1], s4[:, :, :, 3:4], ALU.min))
    dv(lambda e: e.tensor_reduce(t2, pmn, AX.X, ALU.max))
    dv(lambda e: e.tensor_reduce(t1, s4, AX.X, ALU.max))
    dv(lambda e: e.tensor_tensor(gs, t1, t2, ALU.add))
    dv(lambda e: e.tensor_reduce(v1, gs, AX.X, ALU.max))
    dv(lambda e: e.tensor_tensor(gs, gs, bcast(v1, [128, NS, 4], 2), ALU.is_equal))
    m4 = msk.rearrange("p a (g e) -> p a g e", e=4)
    dv(lambda e: e.tensor_tensor(m4, s4, bcast(t2, [128, NS, 4, 4], 3), ALU.is_ge))
    dv(lambda e: e.tensor_tensor(m4, m4, bcast(gs, [128, NS, 4, 4], 3), ALU.mult))
    dv(lambda e: e.tensor_tensor(msk, msk, prob, ALU.mult))
    dv(lambda e: e.tensor_reduce(v2, msk, AX.X, ALU.add))
    dv(lambda e: e.reciprocal(v2, v2))
    dv(lambda e: e.tensor_tensor(msk, msk, bcast(v2, [128, NS, 16], 2), ALU.mult))
    ps_t, rpt = g.ps[3], g.rps[3]
    for si, (s0, sn) in enumerate(subs):
        P.op("pe", lambda e, si=si, sn=sn: e.transpose(ps_t[0:16, 0:sn], msk[0:sn, si, :], ident[0:sn, 0:sn]), [rrt, rr_], [rpt])
        P.op("act", lambda e, s0=s0, sn=sn: e.copy(wT[:, s0:s0 + sn], ps_t[0:16, 0:sn]), [], [rpt, rwT])
    barrier(P)
    A.off = mark
    wge = [A.bf16([128, 8, 512]) for _ in range(2)]; wue = [A.bf16([128, 8, 512]) for _ in range(2)]; wde = [A.bf16([128, 4, 1024]) for _ in range(2)]
    rwe = [Res(), Res()]
    wbc = A.f32([128, 512]); rwbc = Res()
    sg = A.f32([128, 512]); rsg = Res()
    a1 = A.f32([128, 512]); ra1 = Res()
    act_ = A.bf16([128, 4, 512]); ract = Res()
    esel = A.f32([16, 16, 128]); res_ = Res()
    C.ld(esel, g.esel, res_, "sp")
    pG = g.ps[0:2]; rpG = g.rps[0:2]
    pU = g.ps[2:4]; rpU = g.rps[2:4]
    pY = g.ps[4:6]; rpY = g.rps[4:6]
    pB, rpB = g.ps[6], g.rps[6]
    gi = 0
    yi = 0
    for ex_ in range(16):
        b = ex_ % 2
        P.dma("pool", wge[b], g.wgate[l][ex_ * 1024:(ex_ + 1) * 1024, :].rearrange("(k p) n -> p k n", p=128), writes=[rwe[b]])
        P.dma("pool", wue[b], g.wup[l][ex_ * 1024:(ex_ + 1) * 1024, :].rearrange("(k p) n -> p k n", p=128), writes=[rwe[b]])
        P.dma("pool", wde[b], g.wdown[l][ex_ * 512:(ex_ + 1) * 512, :].rearrange("(k p) n -> p k n", p=128), writes=[rwe[b]])
        for ti, (t0, tn, c) in enumerate(TILES):
            P.mm(pB[:, :tn], esel[:, ex_, :], wT[:, t0:t0 + tn], True, True, [res_, rwT], [rpB])
            P.op("act", lambda e, tn=tn: e.copy(wbc[:, :tn], pB[:, :tn]), [], [rpB, rwbc])
            for fcx in range(4):
                gb = gi % 2
                gi += 1
                for k in range(8):
                    P.mm(pG[gb][:, :tn], wge[b][:, k, fcx * 128:(fcx + 1) * 128], h2[:, k, t0:t0 + tn], k == 0, k == 7, [rwe[b], rh2], [rpG[gb]])
                for k in range(8):
                    P.mm(pU[gb][:, :tn], wue[b][:, k, fcx * 128:(fcx + 1) * 128], h2[:, k, t0:t0 + tn], k == 0, k == 7, [rwe[b], rh2], [rpU[gb]])
                P.op("act", lambda e, gb=gb, tn=tn: e.activation(sg[:, :tn], pG[gb][:, :tn], AF.Silu), [], [rpG[gb], rsg])
                P.op("dve", lambda e, gb=gb, tn=tn: e.tensor_tensor(a1[:, :tn], pU[gb][:, :tn], sg[:, :tn], ALU.mult), [rsg], [rpU[gb], ra1])
                P.op("pool", lambda e, fcx=fcx, tn=tn: e.tensor_tensor(act_[:, fcx, :tn], a1[:, :tn], wbc[:, :tn], ALU.mult), [ra1, rwbc], [ract])
            for oc in range(8):
                yb_ = yi % 2
                yi += 1
                for fcx in range(4):
                    P.mm(pY[yb_][:, :tn], wde[b][:, fcx, oc * 128:(oc + 1) * 128], act_[:, fcx, :tn], fcx == 0, fcx == 3, [rwe[b], ract], [rpY[yb_]])
                P.op("dve", lambda e, yb_=yb_, oc=oc, t0=t0, tn=tn, c=c: e.scalar_tensor_tensor(g.x_sb[:, oc, t0:t0 + tn], pY[yb_][:, :tn], g.mod[:, 40 + oc, c:c + 1], g.x_sb[:, oc, t0:t0 + tn], ALU.mult, ALU.add),
                     [g.rmod], [rpY[yb_], g.rx])
    barrier(P)


def s3_host_inputs(inp, core, layers):
    m = {}
    m["wrouter"] = _c(inp["w_router"])
    m["brouter"] = _c(np.tile(np.asarray(inp["b_router"])[None, :], (128, 1)))
    m["ident"] = np.eye(128, dtype=np.float32)
    es = np.zeros((16, 16, 128), np.float32)
    for e in range(16):
        es[e, e, :] = 1.0
    m["esel"] = es
    return m


WSPEC = [
    ("Wfm", 1024, FM_ROWS), ("Wtm", 1024, TM_COLS), ("Wg", 1024, 3072), ("Wada", 1024, 6144),
    ("wpa", 512, 1024), ("wpb", 512, 1024), ("wpc", 512, 1024), ("wout", 1024, 1024),
    ("wgate", 16 * 1024, 512), ("wup", 16 * 1024, 512), ("wdown", 16 * 512, 1024),
]


def build_full(layers=(0, 1), upto="all", debug=()):
    g = G()
    C = g.C = Ctx(); nc = C.nc; P = g.P = C.P
    g.debug = {}
    g.dbgna = (upto == "na")
    if g.dbgna:
        g.dbg_na = C.dout("dbg_na", [128, 4, 256]); g.dbg_na2 = C.dout("dbg_na2", [128, 256]); g.dbg_na3 = C.dout("dbg_na3", [128, 2, 256]); g.dbg_na4 = C.dout("dbg_na4", [64, 256])
    g.upto = upto
    xT = C.din("xT", [1024, NT]); cvec = C.din("cvec", [1024, 2]); hmask_d = C.din("hmask", [128, 8])
    g.b_ada = [C.din("b_ada%d" % l, [128, 48]) for l in range(2)]
    g.n1w = [C.din("n1w%d" % l, [128, 8]) for l in range(2)]
    g.n2w = [C.din("n2w%d" % l, [128, 8]) for l in range(2)]
    wnames = [w for w in WSPEC if upto == "all" or w[0] in ("Wfm", "Wtm", "Wada")]
    sh_in = {}
    for l in layers:
        for (nm, R, Cc) in wnames:
            sh_in[(nm, l)] = C.din("%s%d" % (nm, l), [R // 8, Cc])
    dt_ = lambda n, s: nc.dram_tensor(n, list(s), F32).ap()
    for (nm, R, Cc) in wnames:
        setattr(g, nm, {})
    g.Pfm_mine = dt_("Pfm_mine", [FM_ROWS, NT]); g.Pfm_g = dt_("Pfm_g", [8 * FM_ROWS, NT])
    g.Ptm_mine = dt_("Ptm_mine", [NT, TM_COLS]); g.Ptm_g = dt_("Ptm_g", [8 * NT, TM_COLS])
    g.S2in_fm = dt_("S2in_fm", [960, N]); g.S2in_tm = dt_("S2in_tm", [N, 576])
    g.rsel = Res()
    g.Ymine = dt_("Ymine", [384, N]); g.rY = Res()
    g.Yg = dt_("Yg", [8 * 384, N]); g.S3in_y = dt_("S3in_y", [1536, NT])
    if upto == "all":
        g.wrouter = C.din("wrouter", [1024, 16]); g.brouter = C.din("brouter", [128, 16]); g.ident = C.din("ident", [128, 128])
        g.esel = C.din("esel", [16, 16, 128])
    if upto in ("s2", "all", "na"):
        g.pvec_d = [C.din("pvec%d" % l, [128, 5]) for l in range(2)]
        g.wsink_d = [C.din("wsink%d" % l, [128, 2]) for l in range(2)]
        g.natab = [C.din("natab%d" % l, [2, 3, 128, 8, 512]) for l in range(2)]
        g.ropeC = C.din("ropeC", [64, N]); g.ropeS = C.din("ropeS", [64, N]); g.ropeR_d = C.din("ropeR", [64, 64])
        g.wamask = C.din("wamask", [128, 2, 128]); g.tri = C.din("tri", [64, 6, 64]); g.hind = C.din("hind", [128, 2, 2, 64])
        g.hglow = C.din("hglow", [128, 2, 2]); g.hglow_row = C.din("hglow_row", [64, 2, 2, 128])
    A = g.A = Arena(C, 50400)
    g.ps = [C.ps() for _ in range(8)]; g.rps = [Res() for _ in range(8)]
    g.x_sb = A.f32([128, 8, NT]); g.rx = Res()
    g.mod = A.f32([128, 48, 2]); g.rmod = Res()
    g.A1 = A.f32([128, 8, 2]); g.B1 = A.f32([128, 8, 2]); g.A2 = A.f32([128, 8, 2]); g.B2 = A.f32([128, 8, 2]); g.rAB = Res()
    g.ones = A.f32([128, 128]); g.eps = A.f32([128, 1]); g.hmask = A.f32([128, 8]); g.cv = A.f32([128, 8, 2])
    g.rconst = Res(); g.rcv = Res()
    g.zcol = A.f32([128, 1]); g.pvec = A.f32([128, 5]); g.esink = A.f32([128, 2]); g.ropeR = A.f32([64, 64]); g.rpv = Res()
    A.base = A.off
    P.op("pool", lambda e: e.memset(g.ones, 1.0), [], [g.rconst])
    P.op("pool", lambda e: e.memset(g.eps, RMS_EPS), [], [g.rconst])
    C.ld(g.hmask, hmask_d, g.rconst, "sp")
    C.ld(g.cv, cvec.rearrange("(k p) c -> p k c", p=128), g.rcv, "sp")
    xv = xT.rearrange("(k p) t -> p k t", p=128)
    for k in range(8):
        C.ld(g.x_sb[:, k, :], xv[:, k, :], g.rx)
    full_t = {nm: dt_("%s_full" % nm, [R, Cc]) for (nm, R, Cc) in wnames}
    shard_t = {nm: dt_("%s_sh" % nm, [R // 8, Cc]) for (nm, R, Cc) in wnames}
    for l in layers:
        for wi_, (nm, R, Cc) in enumerate(wnames):
            r1, r2 = Res(), Res()
            P.dma(("sp", "act")[wi_ % 2], shard_t[nm], sh_in[(nm, l)], writes=[r1])
            P.op("pool", lambda e, nm=nm: e.collective_compute("AllGather", ALU.bypass, replica_groups=ALLR, ins=[shard_t[nm]], outs=[full_t[nm]]), [r1], [r2], dma="cc")
            getattr(g, nm)[l] = full_t[nm]
        barrier(P)
        adaln_phase(g, l)
        phase_s1(g, l)
        if upto == "s1":
            break
        if upto == "ag":
            break
        phase_x1(g)
        if upto == "x1":
            break
        phase_s2(g, l)
        if upto in ("s2", "na"):
            break
        barrier(P, ep_switch=True)
        phase_x2(g)
        phase_s3(g, l)
        barrier(P, ep_switch=True)
    if upto == "na":
        pass
    elif upto == "s2":
        o1 = C.dout("dbg_y", [96, N])
        r = Res()
        P.dma("sp", o1, g.Ymine.rearrange("(a b) n -> a b n", b=4)[:, 0, :], writes=[r]); C.outs.append(r)
    elif upto == "s1":
        o1 = C.dout("dbg_fm", [116, NT]); o2 = C.dout("dbg_tm", [132, TM_COLS])
        r = Res()
        P.dma("sp", o1, g.Pfm_mine.rearrange("(a b) n -> a b n", b=32)[:, 0, :], writes=[r]); C.outs.append(r)
        r = Res()
        P.dma("act", o2, g.Ptm_mine.rearrange("(a b) n -> a b n", b=16)[:, 0, :], writes=[r]); C.outs.append(r)
    elif upto == "ag":
        o1 = C.dout("dbg_fm", [8 * 116, NT]); o2 = C.dout("dbg_tm", [8 * 132, TM_COLS])
        r = Res()
        P.dma("sp", o1, g.Pfm_g.rearrange("(a b) n -> a b n", b=32)[:, 0, :], writes=[r]); C.outs.append(r)
        r = Res()
        P.dma("act", o2, g.Ptm_g.rearrange("(a b) n -> a b n", b=16)[:, 0, :], writes=[r]); C.outs.append(r)
    elif upto == "x1":
        o1 = C.dout("dbg_fm", [30, N]); o2 = C.dout("dbg_tm", [528, 576])
        r = Res()
        P.dma("sp", o1, g.S2in_fm.rearrange("(a b) n -> a b n", b=32)[:, 0, :], writes=[r]); C.outs.append(r)
        r = Res()
        P.dma("act", o2, g.S2in_tm.rearrange("(a b) n -> a b n", b=16)[:, 0, :], writes=[r]); C.outs.append(r)
    else:
        if debug:
            o1 = C.dout("dbg_y", [96, N])
            r = Res()
            P.dma("sp", o1, g.Ymine.rearrange("(a b) n -> a b n", b=4)[:, 0, :], writes=[r]); C.outs.append(r)
        xo = C.dout("xo", [1024, NT])
        xov = xo.rearrange("(k p) t -> p k t", p=128)
        for k in range(8):
            C.store(xov[:, k, :], g.x_sb[:, k, :], g.rx)
    return C.finish()


def host_inputs(inp, layers=(0, 1), upto="all"):
    x, c, ctx, c_ctx = [np.asarray(inp[k], np.float32) for k in ("x", "c", "ctx", "c_ctx")]
    wnames = [w for w in WSPEC if upto == "all" or w[0] in ("Wfm", "Wtm", "Wada")]
    full = {}
    for l in layers:
        w_in = np.asarray(inp["w_in"][l], np.float32)
        full[("Wfm", l)] = w_in[:, FM_IDX]; full[("Wtm", l)] = w_in[:, TM_IDX]; full[("Wg", l)] = w_in[:, 4864:]
        full[("Wada", l)] = np.asarray(inp["w_ada"][l], np.float32)
        if upto == "all":
            full[("wpa", l)] = inp["w_pa"][l]; full[("wpb", l)] = inp["w_pb"][l]; full[("wpc", l)] = inp["w_pc"][l]
            full[("wout", l)] = inp["w_out"][l]
            full[("wgate", l)] = np.asarray(inp["w_gate"][l]).reshape(16 * 1024, 512)
            full[("wup", l)] = np.asarray(inp["w_up"][l]).reshape(16 * 1024, 512)
            full[("wdown", l)] = np.asarray(inp["w_down"][l]).reshape(16 * 512, 1024)
    maps = []
    for core in range(NCORES):
        b, j = core // 4, core % 4
        xs = np.concatenate([ctx[b, 64 * j:64 * j + 64], x[b, 2048 * j:2048 * j + 2048]], 0)
        hm = np.zeros((128, 8), np.float32); hm[:, core] = 1.0
        m = {"xT": _c(xs.T), "cvec": _c(np.stack([c_ctx, c[b]], 1)), "hmask": hm}
        for l in range(2):
            m["b_ada%d" % l] = chunkT(inp["b_ada"][l], 48); m["n1w%d" % l] = chunkT(inp["norm1"][l], 8); m["n2w%d" % l] = chunkT(inp["norm2"][l], 8)
        for l in layers:
            for (nm, R, Cc) in wnames:
                rr = R // 8
                m["%s%d" % (nm, l)] = _c(np.asarray(full[(nm, l)])[core * rr:(core + 1) * rr])
        if upto in ("s2", "all", "na"):
            m.update(s2_host_inputs(inp, core, (0, 1)))
        if upto == "all":
            m.update(s3_host_inputs(inp, core, layers))
        maps.append(m)
    return maps


def _c(a):
    return np.ascontiguousarray(a, dtype=np.float32)


def chunkT(v, nch):
    return _c(np.asarray(v).reshape(nch, 128).T)


def run_spmd(nc, in_maps):
    res = run_bass_kernel_spmd(nc, in_maps, core_ids=list(range(len(in_maps))))
    return res.results


def kernel(**inp):
    nc = build_full(layers=(0, 1), upto="all")
    maps = host_inputs(inp, layers=(0, 1), upto="all")
    res = run_spmd(nc, maps)
    x = np.asarray(inp["x"])
    out = np.empty(x.shape, np.float32)
    for core in range(NCORES):
        b, j = core // 4, core % 4
        out[b, 2048 * j:2048 * j + 2048] = res[core]["xo"][:, NCTX:].T
    return out
```
